# Optimizing a Trainium2 kernel written in Bass

```python
import jax, jax.numpy as jnp
from jax import lax
import numpy as np

D_MODEL = 2048
BATCH = 2
SEQ = 16384
DEPTH = 1

HEAD_DIM = 128
N_ATTN_HEADS = D_MODEL // (2 * HEAD_DIM)
ATTN_W = N_ATTN_HEADS * HEAD_DIM
N_MEM_HEADS = 4
MEM_W = N_MEM_HEADS * HEAD_DIM
CONV_W = D_MODEL - ATTN_W - MEM_W
CONV_K = 3
N_MEM = 256
DILATED_BRANCHES = ((128, 1), (512, 4), (2048, 16))
ATTN_BLOCK = 128
PROJ_W = 3 * ATTN_W + 3 * CONV_W + MEM_W
SPLITS = (ATTN_W, 2 * ATTN_W, 3 * ATTN_W, 3 * ATTN_W + CONV_W,
          3 * ATTN_W + 2 * CONV_W, 3 * ATTN_W + 3 * CONV_W)
N_GROUPS = 8
EXPERTS_PER_GROUP = 8
N_EXPERTS = N_GROUPS * EXPERTS_PER_GROUP
TOP_K_IN_GROUP = 2
D_EXPERT = D_MODEL // 2
MOE_BLOCK = 128
EPS = 1e-6
NEG_INF = -1e30

kernel_name = "hymba_dilated_conv_memory_hmoe"


def rms_norm(x, g):
    xf = x.astype(jnp.float32)
    y = xf * lax.rsqrt(jnp.mean(xf * xf, axis=-1, keepdims=True) + EPS)
    return (y * g.astype(jnp.float32)).astype(x.dtype)


def alibi_slopes(n):
    return 2.0 ** (-8.0 * jnp.arange(1, n + 1, dtype=jnp.float32) / n)


def banded_causal_attention(q, k, v, span, step, slopes):
    N, L, H, hd = q.shape
    blk = ATTN_BLOCK
    nb = -(-L // blk)
    Lp = nb * blk
    pad = ((0, 0), (0, Lp - L), (0, 0), (0, 0))
    qb = jnp.pad(q, pad).reshape(N, nb, blk, H, hd).astype(jnp.float32)
    kb = jnp.pad(k, pad).reshape(N, nb, blk, H, hd).astype(jnp.float32)
    vb = jnp.pad(v, pad).reshape(N, nb, blk, H, hd).astype(jnp.float32)
    shift = ((0, 0), (1, 0), (0, 0), (0, 0), (0, 0))
    kk = jnp.concatenate([jnp.pad(kb, shift)[:, :-1], kb], axis=2)
    vv = jnp.concatenate([jnp.pad(vb, shift)[:, :-1], vb], axis=2)
    s = jnp.einsum('nbqhd,nbkhd->nbhqk', qb, kk) * (hd ** -0.5)
    qpos = jnp.arange(nb)[:, None] * blk + jnp.arange(blk)[None, :]
    kpos = jnp.arange(nb)[:, None] * blk - blk + jnp.arange(2 * blk)[None, :]
    dist = qpos[:, :, None] - kpos[:, None, :]
    valid = (dist >= 0) & (dist <= span) & (kpos[:, None, :] >= 0)
    bias = -slopes[None, :, None, None] * (dist * step).astype(jnp.float32)[:, None]
    s = jnp.where(valid[None, :, None], s + bias[None], NEG_INF)
    lse = jax.nn.logsumexp(s, axis=-1)
    p = jnp.exp(s - lse[..., None])
    o = jnp.einsum('nbhqk,nbkhd->nbqhd', p, vv).reshape(N, Lp, H, hd)[:, :L]
    lse = jnp.transpose(lse, (0, 1, 3, 2)).reshape(N, Lp, H)[:, :L]
    return o, lse


def dilated_attention(q, k, v, slopes):
    B, S, H, hd = q.shape
    outs, lses = [], []
    for window, dil in DILATED_BRANCHES:
        Ls = S // dil
        def to_res(t):
            return t.reshape(B, Ls, dil, H, hd).transpose(0, 2, 1, 3, 4).reshape(B * dil, Ls, H, hd)
        o, lse = banded_causal_attention(to_res(q), to_res(k), to_res(v), window // dil, dil, slopes)
        outs.append(o.reshape(B, dil, Ls, H, hd).transpose(0, 2, 1, 3, 4).reshape(B, S, H, hd))
        lses.append(lse.reshape(B, dil, Ls, H).transpose(0, 2, 1, 3).reshape(B, S, H))
    w = jax.nn.softmax(jnp.stack(lses, axis=0), axis=0)
    return jnp.sum(w[..., None] * jnp.stack(outs, axis=0), axis=0)


def gated_short_conv(b_gate, c_gate, u, conv_w):
    z = c_gate * u
    C = z.shape[-1]
    y = lax.conv_general_dilated(z, conv_w[:, None, :].astype(z.dtype), window_strides=(1,),
                                 padding=((CONV_K - 1, 0),),
                                 dimension_numbers=('NWC', 'WIO', 'NWC'),
                                 feature_group_count=C)
    return b_gate * y


def memory_attention(q, mk, mv):
    s = jnp.einsum('bshd,bmhd->bhsm', q.astype(jnp.float32), mk.astype(jnp.float32)) * (HEAD_DIM ** -0.5)
    p = jax.nn.softmax(s, axis=-1)
    return jnp.einsum('bhsm,bmhd->bshd', p, mv.astype(jnp.float32))


def hierarchical_moe(h, w_rg, b_rg, w_re, b_re, w_gate, w_up, w_down):
    T, D = h.shape
    hf = h.astype(jnp.float32)
    g_prob = jax.nn.softmax(hf @ w_rg.astype(jnp.float32) + b_rg.astype(jnp.float32), axis=-1)
    g_w, g_idx = lax.top_k(g_prob, 1)
    e_logits = (hf @ w_re.astype(jnp.float32) + b_re.astype(jnp.float32)).reshape(T, N_GROUPS, EXPERTS_PER_GROUP)
    e_logits = jnp.take_along_axis(e_logits, g_idx[:, :, None], axis=1)[:, 0]
    e_prob = jax.nn.softmax(e_logits, axis=-1)
    top_p, top_i = lax.top_k(e_prob, TOP_K_IN_GROUP)
    top_p = top_p / jnp.sum(top_p, axis=-1, keepdims=True)
    gates = (g_w * top_p).reshape(-1)
    experts = (g_idx * EXPERTS_PER_GROUP + top_i).reshape(-1)
    tokens = jnp.repeat(jnp.arange(T), TOP_K_IN_GROUP)
    A = T * TOP_K_IN_GROUP
    order = jnp.argsort(experts)
    e_sorted, tok_sorted, gate_sorted = experts[order], tokens[order], gates[order]
    counts = jnp.bincount(experts, length=N_EXPERTS)
    padded = (counts + MOE_BLOCK - 1) // MOE_BLOCK * MOE_BLOCK
    starts = jnp.cumsum(counts) - counts
    pends = jnp.cumsum(padded)
    pstarts = pends - padded
    dest = pstarts[e_sorted] + (jnp.arange(A) - starts[e_sorted])
    n_blocks = (A + N_EXPERTS * (MOE_BLOCK - 1) + MOE_BLOCK - 1) // MOE_BLOCK
    R = n_blocks * MOE_BLOCK
    xbuf = jnp.zeros((R, D), h.dtype).at[dest].set(h[tok_sorted])
    block_expert = jnp.minimum(
        jnp.searchsorted(pends, jnp.arange(n_blocks) * MOE_BLOCK, side='right'), N_EXPERTS - 1)

    def expert_block(args):
        xb, e = args
        a = xb @ w_gate[e]
        u = xb @ w_up[e]
        return (jax.nn.silu(a) * u) @ w_down[e]

    ybuf = lax.map(expert_block, (xbuf.reshape(n_blocks, MOE_BLOCK, D), block_expert)).reshape(R, D)
    y = ybuf[dest] * gate_sorted[:, None].astype(ybuf.dtype)
    return jax.ops.segment_sum(y, tok_sorted, num_segments=T)


def hybrid_layer(x, mem, norm1_g, w_in, q_norm_g, k_norm_g, conv_w, mem_norm_g, w_mem_kv,
                 mem_q_norm_g, mem_k_norm_g, out_norm_g, w_out, norm2_g,
                 w_rg, b_rg, w_re, b_re, w_gate, w_up, w_down):
    B, S, D = x.shape
    h = rms_norm(x, norm1_g)
    proj = jnp.einsum('bsd,de->bse', h, w_in)
    q, k, v, b_gate, c_gate, u, mq = jnp.split(proj, SPLITS, axis=-1)
    q = rms_norm(q.reshape(B, S, N_ATTN_HEADS, HEAD_DIM), q_norm_g)
    k = rms_norm(k.reshape(B, S, N_ATTN_HEADS, HEAD_DIM), k_norm_g)
    v = v.reshape(B, S, N_ATTN_HEADS, HEAD_DIM)
    attn_o = dilated_attention(q, k, v, alibi_slopes(N_ATTN_HEADS)).reshape(B, S, ATTN_W).astype(x.dtype)
    conv_o = gated_short_conv(b_gate, c_gate, u, conv_w)
    mem_h = rms_norm(mem, mem_norm_g)
    mk, mv = jnp.split(jnp.einsum('bmd,de->bme', mem_h, w_mem_kv), 2, axis=-1)
    mk = rms_norm(mk.reshape(B, N_MEM, N_MEM_HEADS, HEAD_DIM), mem_k_norm_g)
    mv = mv.reshape(B, N_MEM, N_MEM_HEADS, HEAD_DIM)
    mq = rms_norm(mq.reshape(B, S, N_MEM_HEADS, HEAD_DIM), mem_q_norm_g)
    mem_o = memory_attention(mq, mk, mv).reshape(B, S, MEM_W).astype(x.dtype)
    y = jnp.concatenate([rms_norm(attn_o, out_norm_g[:ATTN_W]),
                         rms_norm(conv_o, out_norm_g[ATTN_W:ATTN_W + CONV_W]),
                         rms_norm(mem_o, out_norm_g[ATTN_W + CONV_W:])], axis=-1)
    x = x + jnp.einsum('bse,ed->bsd', y, w_out)
    h2 = rms_norm(x, norm2_g).reshape(B * S, D)
    x = x + hierarchical_moe(h2, w_rg, b_rg, w_re, b_re, w_gate, w_up, w_down).reshape(B, S, D).astype(x.dtype)
    return x


def setup_inputs(seed: int = 0) -> dict:
    key = jax.random.key(seed)
    ks = jax.random.split(key, 24)
    f32 = jnp.float32
    nrm = lambda k, shape, scale: jax.random.normal(k, shape, f32) * scale
    gain = lambda k, shape: 1.0 + 0.05 * jax.random.normal(k, shape, f32)
    L = DEPTH
    return {
        "x": jax.random.normal(ks[0], (BATCH, SEQ, D_MODEL), f32),
        "mem": jax.random.normal(ks[1], (BATCH, N_MEM, D_MODEL), f32),
        "norm1_g": gain(ks[2], (L, D_MODEL)),
        "w_in": nrm(ks[3], (L, D_MODEL, PROJ_W), D_MODEL ** -0.5),
        "q_norm_g": gain(ks[4], (L, HEAD_DIM)),
        "k_norm_g": gain(ks[5], (L, HEAD_DIM)),
        "conv_w": nrm(ks[6], (L, CONV_K, CONV_W), CONV_K ** -0.5),
        "mem_norm_g": gain(ks[7], (L, D_MODEL)),
        "w_mem_kv": nrm(ks[8], (L, D_MODEL, 2 * MEM_W), D_MODEL ** -0.5),
        "mem_q_norm_g": gain(ks[9], (L, HEAD_DIM)),
        "mem_k_norm_g": gain(ks[10], (L, HEAD_DIM)),
        "out_norm_g": gain(ks[11], (L, D_MODEL)),
        "w_out": nrm(ks[12], (L, D_MODEL, D_MODEL), D_MODEL ** -0.5),
        "norm2_g": gain(ks[13], (L, D_MODEL)),
        "w_router_group": nrm(ks[14], (L, D_MODEL, N_GROUPS), D_MODEL ** -0.5),
        "b_router_group": nrm(ks[15], (L, N_GROUPS), 0.01),
        "w_router_expert": nrm(ks[16], (L, D_MODEL, N_EXPERTS), D_MODEL ** -0.5),
        "b_router_expert": nrm(ks[17], (L, N_EXPERTS), 0.01),
        "w_gate": nrm(ks[18], (L, N_EXPERTS, D_MODEL, D_EXPERT), D_MODEL ** -0.5),
        "w_up": nrm(ks[19], (L, N_EXPERTS, D_MODEL, D_EXPERT), D_MODEL ** -0.5),
        "w_down": nrm(ks[20], (L, N_EXPERTS, D_EXPERT, D_MODEL), D_EXPERT ** -0.5),
    }


def reference(x, mem, norm1_g, w_in, q_norm_g, k_norm_g, conv_w, mem_norm_g, w_mem_kv,
              mem_q_norm_g, mem_k_norm_g, out_norm_g, w_out, norm2_g,
              w_router_group, b_router_group, w_router_expert, b_router_expert,
              w_gate, w_up, w_down):
    for l in range(DEPTH):
        x = hybrid_layer(x, mem, norm1_g[l], w_in[l], q_norm_g[l], k_norm_g[l], conv_w[l],
                         mem_norm_g[l], w_mem_kv[l], mem_q_norm_g[l], mem_k_norm_g[l],
                         out_norm_g[l], w_out[l], norm2_g[l],
                         w_router_group[l], b_router_group[l], w_router_expert[l], b_router_expert[l],
                         w_gate[l], w_up[l], w_down[l])
    return x
```

```python
import numpy as np
import ml_dtypes
from contextlib import ExitStack
import concourse.bass as bass
import concourse.mybir as mybir
from concourse.bass_utils import run_bass_kernel_spmd

F32 = mybir.dt.float32
BF16 = mybir.dt.bfloat16
I32 = mybir.dt.int32
AF = mybir.ActivationFunctionType
ALU = mybir.AluOpType
AX = mybir.AxisListType
BF = ml_dtypes.bfloat16

NCORE = 8
D = 2048
TOWN = 4096
HALO = 2048
TLOC = TOWN + HALO
NT = TOWN // 128
EPS = 1e-6
NBLK = 128
RBUF = NBLK * 128
SC = 128.0 ** -0.5
BRANCH = (1, 4, 16)


def ssl(start, count, step=1):
    return slice(start, start + step * (count - 1) + 1, step)


class _Stop(Exception):
    pass


class TB:
    __slots__ = ("w", "r")

    def __init__(self):
        self.w = None
        self.r = {}


class T:
    def __init__(self, t):
        self.t = t
        self.b = TB()


class Prog:
    ENG = ("pe", "act", "dve", "pool", "sp")

    def __init__(self, nc, stack, ndma=24, same_engine_sync=True):
        self.nc = nc
        self.ndma = ndma
        self.same = same_engine_sync
        self.sems = {}
        for e in ("pe", "act", "dve", "pool"):
            self.sems[e] = stack.enter_context(nc.semaphore("c_" + e))
        for i in range(ndma):
            self.sems[("d", i)] = stack.enter_context(nc.semaphore("d%d" % i))
        self.cnt = {e: 0 for e in ("pe", "act", "dve", "pool")}
        self.dcnt = [0] * ndma
        self.rr = 0
        self.known = {e: {} for e in self.ENG}
        self.streams = {e: [] for e in self.ENG}

    def _deps(self, eng, reads, writes, extra=()):
        need = {}

        def add(tok):
            k, v = tok
            if k == "pe" and eng == "pe":
                return
            if k == eng and not self.same:
                return
            if self.known[eng].get(k, 0) >= v:
                return
            if need.get(k, 0) < v:
                need[k] = v

        for b in reads:
            if b.w is not None:
                add(b.w)
        for b in writes:
            if b.w is not None:
                add(b.w)
            for k, v in b.r.items():
                add((k, v))
        for t in extra:
            add(t)
        for k, v in need.items():
            self.known[eng][k] = v
            self.streams[eng].append(("wait", k, v))

    def _commit(self, tok, reads, writes):
        k, v = tok
        for b in reads:
            if b.r.get(k, 0) < v:
                b.r[k] = v
        for b in writes:
            b.w = tok
            b.r = {}

    def op(self, eng, fn, reads=(), writes=()):
        reads = [x.b if isinstance(x, T) else x for x in reads]
        writes = [x.b if isinstance(x, T) else x for x in writes]
        self._deps(eng, reads, writes)
        self.cnt[eng] += 1
        tok = (eng, self.cnt[eng])
        self.streams[eng].append(("op", fn, eng))
        self._commit(tok, reads, writes)
        return tok

    def dma(self, q, fn, reads=(), writes=()):
        reads = [x.b if isinstance(x, T) else x for x in reads]
        writes = [x.b if isinstance(x, T) else x for x in writes]
        s = self.rr
        self.rr = (s + 1) % self.ndma
        extra = []
        if self.dcnt[s] > 0:
            extra.append((("d", s), self.dcnt[s]))
        self._deps(q, reads, writes, extra)
        self.dcnt[s] += 16
        tok = (("d", s), self.dcnt[s])
        self.streams[q].append(("dma", fn, ("d", s)))
        self._commit(tok, reads, writes)
        return tok

    def reset_sems(self):
        sems = list(self.sems.values())
        with self.nc.Block() as block:
            def f(e):
                for s_ in sems:
                    e.sem_clear(s_)
            block.gpsimd(f)

    def end_phase(self):
        for e in ("sp", "pool"):
            for s in range(self.ndma):
                if self.dcnt[s] > 0 and self.known[e].get(("d", s), 0) < self.dcnt[s]:
                    self.known[e][("d", s)] = self.dcnt[s]
                    self.streams[e].append(("wait", ("d", s), self.dcnt[s]))
        self.flush()

    def flush(self):
        nc = self.nc
        engobj = {"pe": "tensor", "act": "scalar", "dve": "vector", "pool": "gpsimd", "sp": "sync"}
        sems = self.sems

        def run(e, lst):
            for it in lst:
                if it[0] == "wait":
                    e.wait_ge(sems[it[1]], it[2])
                elif it[0] == "op":
                    it[1](e).then_inc(sems[it[2]], 1)
                else:
                    it[1](e).then_inc(sems[it[2]], 16)

        with nc.Block() as block:
            for en in self.ENG:
                lst = self.streams[en]
                if lst:
                    getattr(block, engobj[en])(lambda e, lst=lst: run(e, lst))
        self.streams = {e: [] for e in self.ENG}


def build_nc(upto="M4", debug=False):
    nc = bass.Bass("TRN2", target_bir_lowering=False)
    PH = ["A1", "A2", "A3", "M2", "M3", "M4"]
    lvl = PH.index(upto)

    def din(name, shape, dt=F32):
        return nc.dram_tensor(name, list(shape), dt, kind="ExternalInput").ap()

    def dscr(name, shape, dt):
        return nc.dram_tensor(name, list(shape), dt, kind=("ExternalOutput" if (debug and name in debug) else "Internal")).ap()

    xh = din("xh", [TLOC, D])
    memx = din("memx", [256, D])
    g1bc = din("g1bc", [128, D])
    gmbc = din("gmbc", [128, D])
    g2bc = din("g2bc", [128, D])
    brbc = din("brbc", [128, 72])
    gcols = din("gcols", [128, 4])
    goutc = din("goutc", [128, 16])
    convc = din("convc", [128, 12])
    w_in = din("w_in", [D, 5120])
    w_mem = din("w_mem", [D, 1024])
    w_out = din("w_out", [D, D])
    w_r = din("w_r", [D, 72])
    if lvl >= 4:
        wg = [din("wg%d" % c, [16384, 2048]) for c in range(4)]
        wu = [din("wu%d" % c, [16384, 2048]) for c in range(4)]
        wd = [din("wd%d" % c, [16384, 2048]) for c in range(4)]
    identd = din("ident", [128, 128], BF16)
    triud = din("triu", [128, 128], BF16)
    biasd = din("biasS", [128, 48 * 128], BF16)
    kinvd = din("kinv", [1, TLOC], BF16)
    base8d = din("base8", [128, 8])
    out = nc.dram_tensor("out", [TOWN, D], F32, kind="ExternalOutput").ap()

    KT = dscr("KT", [8, 128, TLOC], BF16)
    VT = dscr("VT", [8, 128, TLOC], BF16)
    QT = dscr("QT", [8, 128, TOWN], BF16)
    YCM = dscr("YCM", [8, 128, TOWN], BF16)
    ATT = dscr("ATT", [8, 128, TOWN], BF16)
    X1 = dscr("X1", [TOWN, D], F32)
    H2 = dscr("H2", [TOWN, D], BF16)
    XBUF = dscr("XBUF", [RBUF, D], BF16)
    YBUF = dscr("YBUF", [RBUF, D], F32)

    try:
      with ExitStack() as top:
        P = Prog(nc, top)
        P.reset_sems()
        dbg_out = {}

        def stop_if(k):
            if lvl != k:
                return
            if not debug:
                P.reset_sems()
            if debug:
                for nm, t_ in dbg_out.items():
                    if nm not in debug:
                        continue
                    shp = list(t_.t.shape)
                    d_ = nc.dram_tensor("dbg_" + nm, shp, t_.t.dtype, kind="ExternalOutput").ap()
                    P.dma("sp", lambda e, d_=d_, t_=t_: e.dma_start(out=d_, in_=t_.t[:]), reads=[t_], writes=[TB()])
                P.end_phase()
            raise _Stop()

        def mk(st, name, shape, dt, psum=False):
            f = nc.psum_tensor if psum else nc.sbuf_tensor
            return T(st.enter_context(f(name, list(shape), dt)))

        ident = mk(top, "ident_s", [128, 128], BF16)
        ones = mk(top, "ones_s", [128, 128], BF16)
        gcol = mk(top, "gcol_s", [128, 4], F32)
        gout = mk(top, "gout_s", [128, 16], F32)
        ssA = mk(top, "ssA_s", [128, NT], F32)
        gates = mk(top, "gates_s", [128, NT, 2], F32)
        desti = mk(top, "desti_s", [128, NT, 2], I32)
        idxW = mk(top, "idxW", [128, NBLK, 8], I32)
        dbg_out["idxW"] = idxW
        P.dma("sp", lambda e: e.dma_start(out=ident.t[:], in_=identd), writes=[ident])
        P.dma("sp", lambda e: e.dma_start(out=gcol.t[:], in_=gcols), writes=[gcol])
        P.dma("sp", lambda e: e.dma_start(out=gout.t[:], in_=goutc), writes=[gout])
        P.op("pool", lambda e: e.memset(ones.t[:], 1.0), writes=[ones])
        dbg_out.update(ssA=ssA, gates=gates, desti=desti)

        def rstd_of(ss_ap, ssT, tmp, tmp_ap, out, out_ap, scale):
            P.op("act", lambda e: e.activation(out=tmp_ap, in_=ss_ap, func=AF.Sqrt, bias=EPS, scale=scale),
                 reads=[ssT], writes=[tmp])
            P.op("dve", lambda e: e.reciprocal(out=out_ap, in_=tmp_ap), reads=[tmp], writes=[out])

        def cast_load_w(Wt, src, ncols, col0, dcol0):
            step = 1024
            for k in range(16):
                for c in range(0, ncols, step):
                    n = min(step, ncols - c)
                    P.dma("pool", lambda e, k=k, c=c, n=n: e.dma_start(
                        out=Wt.t[:, k, dcol0 + c:dcol0 + c + n],
                        in_=src[k * 128:(k + 1) * 128, col0 + c:col0 + c + n]), writes=[Wt])

        with ExitStack() as st:
            gbc = mk(st, "gbc", [128, D], F32)
            xt = [mk(st, "xt%d" % i, [128, D], F32) for i in range(2)]
            junk = mk(st, "junk", [128, D], BF16)
            ss = mk(st, "ss", [128, 1], F32)
            tmp1 = mk(st, "tmp1", [128, 1], F32)
            rstd = mk(st, "rstd", [128, 1], F32)
            xn = mk(st, "xn", [128, D], BF16)
            xnT = mk(st, "xnT", [128, 16, 512], BF16)
            W = mk(st, "W", [128, 16, 3072], BF16)
            mkT = mk(st, "mkT", [128, 4, 256], BF16)
            mv = mk(st, "mv", [128, 2, 512], BF16)
            sq = mk(st, "sq", [128, 512], BF16)
            tf = mk(st, "tf", [128, 512], F32)
            rf = mk(st, "rf", [128, 512], F32)
            ob = [mk(st, "ob%d" % i, [128, 512], BF16) for i in range(2)]
            csb = mk(st, "csb", [128, 512], F32)
            zb = [mk(st, "zb%d" % i, [128, 514], F32) for i in range(4)]
            t1 = mk(st, "t1", [128, 512], F32)
            yc = [mk(st, "yc%d" % i, [128, 512], F32) for i in range(4)]
            pTm = mk(st, "pTm", [128, 2, 512], BF16)
            cw = mk(st, "cw", [128, 12], F32)
            pT = mk(st, "pT", [128, 2048], BF16, psum=True)
            pp = [mk(st, "pp%d" % i, [128, 512], F32, psum=True) for i in range(3)]
            p2 = mk(st, "p2", [128, 512], F32, psum=True)
            pS = [mk(st, "pS%d" % i, [128, 512], F32, psum=True) for i in range(2)]
            P.dma("sp", lambda e: e.dma_start(out=cw.t[:], in_=convc), writes=[cw])
            for i in range(4):
                P.op("pool", lambda e, i=i: e.memset(zb[i].t[:], 0.0), writes=[zb[i]])
            cnt = {"x": 0, "pp": 0, "ob": 0}

            def front(rows_ap_fn, ntiles):
                for ti in range(ntiles):
                    x_ = xt[cnt["x"] % 2]
                    cnt["x"] += 1
                    P.dma("sp", lambda e, x_=x_, ti=ti: e.dma_start(out=x_.t[:], in_=rows_ap_fn(ti)), writes=[x_])
                    P.op("act", lambda e, x_=x_: e.activation(out=junk.t[:], in_=x_.t[:], func=AF.Square,
                                                              accum_out=ss.t[:]), reads=[x_], writes=[junk, ss])
                    rstd_of(ss.t[:], ss, tmp1, tmp1.t[:], rstd, rstd.t[:], 1.0 / D)
                    P.op("dve", lambda e, x_=x_: e.scalar_tensor_tensor(
                        out=xn.t[:], in0=x_.t[:], scalar=rstd.t[:, 0:1], in1=gbc.t[:], op0=ALU.mult, op1=ALU.mult),
                        reads=[x_, rstd, gbc], writes=[xn])

                    def tr(e):
                        for k in range(16):
                            i_ = e.transpose(out=pT.t[:, k * 128:(k + 1) * 128], in_=xn.t[:, k * 128:(k + 1) * 128],
                                             identity=ident.t[:])
                        return i_
                    P.op("pe", tr, reads=[xn, ident], writes=[pT])
                    P.op("act", lambda e, ti=ti: e.activation(
                        out=xnT.t[:, 0:8, ti * 128:(ti + 1) * 128],
                        in_=pT.t[:, 0:1024].rearrange("p (k n) -> p k n", n=128), func=AF.Copy),
                        reads=[pT], writes=[xnT])
                    P.op("dve", lambda e, ti=ti: e.tensor_copy(
                        out=xnT.t[:, 8:16, ti * 128:(ti + 1) * 128],
                        in_=pT.t[:, 1024:2048].rearrange("p (k n) -> p k n", n=128)),
                        reads=[pT], writes=[xnT])

            def proj(col0, ntok=512):
                p_ = pp[cnt["pp"] % 3]
                cnt["pp"] += 1

                def f(e):
                    for k in range(16):
                        i_ = e.matmul(p_.t[:, 0:ntok], lhsT=W.t[:, k, col0:col0 + 128], rhs=xnT.t[:, k, 0:ntok],
                                      start=(k == 0), stop=(k == 15))
                    return i_
                P.op("pe", f, reads=[W, xnT], writes=[p_])
                return p_

            def rep_rstd(srcs, scale, ntok=512):
                n = len(srcs)
                for i, (s_, ap) in enumerate(srcs):
                    P.op("act", lambda e, ap=ap: e.activation(out=sq.t[:, 0:ntok], in_=ap, func=AF.Square),
                         reads=[s_], writes=[sq])
                    P.op("pe", lambda e, i=i: e.matmul(p2.t[:, 0:ntok], lhsT=ones.t[:], rhs=sq.t[:, 0:ntok],
                                                       start=(i == 0), stop=(i == n - 1)),
                         reads=[ones, sq], writes=[p2])
                rstd_of(p2.t[:, 0:ntok], p2, tf, tf.t[:, 0:ntok], rf, rf.t[:, 0:ntok], scale)

            def scaled_out(src, src_ap, gain_ap, gainT, ntok=512):
                o_ = ob[cnt["ob"] % 2]
                cnt["ob"] += 1
                P.op("dve", lambda e: e.scalar_tensor_tensor(
                    out=o_.t[:, 0:ntok], in0=src_ap, scalar=gain_ap, in1=rf.t[:, 0:ntok], op0=ALU.mult,
                    op1=ALU.mult), reads=[src, gainT, rf], writes=[o_])
                return o_

            def store(o_, dst_ap, ntok=512):
                P.dma("sp", lambda e: e.dma_start(out=dst_ap, in_=o_.t[:, 0:ntok]), reads=[o_], writes=[TB()])

            P.dma("sp", lambda e: e.dma_start(out=gbc.t[:], in_=gmbc), writes=[gbc])
            cast_load_w(W, w_mem, 1024, 0, 0)
            front(lambda ti: memx[ti * 128:(ti + 1) * 128, :], 2)
            for hm in range(4):
                p_ = proj(hm * 128, 256)
                rep_rstd([(p_, p_.t[:, 0:256])], 1.0 / 128, 256)
                P.op("dve", lambda e, p_=p_, hm=hm: e.scalar_tensor_tensor(
                    out=mkT.t[:, hm, :], in0=p_.t[:, 0:256], scalar=gcol.t[:, 3:4], in1=rf.t[:, 0:256],
                    op0=ALU.mult, op1=ALU.mult), reads=[p_, gcol, rf], writes=[mkT])
            for mc in range(2):
                p_ = pp[cnt["pp"] % 3]
                cnt["pp"] += 1

                def f(e, p_=p_, mc=mc):
                    for k in range(16):
                        i_ = e.matmul(p_.t[:, :], lhsT=xnT.t[:, k, mc * 128:(mc + 1) * 128], rhs=W.t[:, k, 512:1024],
                                      start=(k == 0), stop=(k == 15))
                    return i_
                P.op("pe", f, reads=[W, xnT], writes=[p_])
                P.op("act", lambda e, p_=p_, mc=mc: e.activation(out=mv.t[:, mc, :], in_=p_.t[:, :], func=AF.Copy),
                     reads=[p_], writes=[mv])

            P.dma("sp", lambda e: e.dma_start(out=gbc.t[:], in_=g1bc), writes=[gbc])
            cast_load_w(W, w_in, 2048, 1024, 0)
            for s_ in range(TLOC // 512):
                front(lambda ti, s_=s_: xh[s_ * 512 + ti * 128:s_ * 512 + (ti + 1) * 128, :], 4)
                for h in range(8):
                    p_ = proj(h * 128)
                    rep_rstd([(p_, p_.t[:, :])], 1.0 / 128)
                    o_ = scaled_out(p_, p_.t[:, :], gcol.t[:, 1:2], gcol)
                    store(o_, KT[h, :, s_ * 512:(s_ + 1) * 512])
                for h in range(8):
                    p_ = proj(1024 + h * 128)
                    o_ = ob[cnt["ob"] % 2]
                    cnt["ob"] += 1
                    P.op("act", lambda e, p_=p_, o_=o_: e.activation(out=o_.t[:, :], in_=p_.t[:, :], func=AF.Copy),
                         reads=[p_], writes=[o_])
                    store(o_, VT[h, :, s_ * 512:(s_ + 1) * 512])

            cast_load_w(W, w_in, 1024, 0, 0)
            cast_load_w(W, w_in, 2048, 3072, 1024)
            for s_ in range(3, TLOC // 512):
                own = s_ >= 4
                oc = (s_ - 4) * 512
                front(lambda ti, s_=s_: xh[s_ * 512 + ti * 128:s_ * 512 + (ti + 1) * 128, :], 4)
                if own:
                    for h in range(8):
                        p_ = proj(h * 128)
                        rep_rstd([(p_, p_.t[:, :])], 1.0 / 128)
                        o_ = scaled_out(p_, p_.t[:, :], gcol.t[:, 0:1], gcol)
                        store(o_, QT[h, :, oc:oc + 512])
                for cc in range(4):
                    pc = proj(1536 + cc * 128)
                    pu = proj(2048 + cc * 128)
                    P.op("act", lambda e, pc=pc: e.activation(out=csb.t[:, :], in_=pc.t[:, :], func=AF.Copy),
                         reads=[pc], writes=[csb])
                    z = zb[cc]
                    P.op("dve", lambda e, z=z, pu=pu: e.tensor_tensor(out=z.t[:, 2:514], in0=csb.t[:, :],
                                                                      in1=pu.t[:, :], op=ALU.mult),
                         reads=[csb, pu], writes=[z])
                    if own:
                        pb = proj(1024 + cc * 128)
                        P.op("dve", lambda e, z=z, cc=cc: e.tensor_scalar(
                            out=t1.t[:, :], in0=z.t[:, 0:512], scalar1=cw.t[:, cc * 3:cc * 3 + 1], scalar2=None,
                            op0=ALU.mult), reads=[z, cw], writes=[t1])
                        P.op("dve", lambda e, z=z, cc=cc: e.scalar_tensor_tensor(
                            out=t1.t[:, :], in0=z.t[:, 1:513], scalar=cw.t[:, cc * 3 + 1:cc * 3 + 2], in1=t1.t[:, :],
                            op0=ALU.mult, op1=ALU.add), reads=[z, cw, t1], writes=[t1])
                        P.op("dve", lambda e, z=z, cc=cc: e.scalar_tensor_tensor(
                            out=t1.t[:, :], in0=z.t[:, 2:514], scalar=cw.t[:, cc * 3 + 2:cc * 3 + 3], in1=t1.t[:, :],
                            op0=ALU.mult, op1=ALU.add), reads=[z, cw, t1], writes=[t1])
                        P.op("dve", lambda e, pb=pb, cc=cc: e.tensor_tensor(out=yc[cc].t[:, :], in0=t1.t[:, :],
                                                                            in1=pb.t[:, :], op=ALU.mult),
                             reads=[t1, pb], writes=[yc[cc]])
                    P.op("dve", lambda e, z=z: e.tensor_copy(out=z.t[:, 0:2], in_=z.t[:, 512:514]),
                         reads=[z], writes=[z])
                if not own:
                    continue
                rep_rstd([(yc[cc], yc[cc].t[:, :]) for cc in range(4)], 1.0 / 512)
                for cc in range(4):
                    o_ = scaled_out(yc[cc], yc[cc].t[:, :], gout.t[:, 8 + cc:9 + cc], gout)
                    store(o_, YCM[cc, :, oc:oc + 512])
                for hm in range(4):
                    p_ = proj(2560 + hm * 128)
                    rep_rstd([(p_, p_.t[:, :])], 1.0 / 128)
                    mq = scaled_out(p_, p_.t[:, :], gcol.t[:, 2:3], gcol)
                    for mc in range(2):
                        P.op("pe", lambda e, mc=mc, hm=hm, mq=mq: e.matmul(
                            pS[mc].t[:, :], lhsT=mkT.t[:, hm, mc * 128:(mc + 1) * 128], rhs=mq.t[:, :],
                            start=True, stop=True), reads=[mkT, mq], writes=[pS[mc]])
                        P.op("act", lambda e, mc=mc: e.activation(out=pTm.t[:, mc, :], in_=pS[mc].t[:, :],
                                                                  func=AF.Exp, scale=SC),
                             reads=[pS[mc]], writes=[pTm])
                    po = pp[cnt["pp"] % 3]
                    cnt["pp"] += 1

                    def fo(e, po=po, hm=hm):
                        for mc in range(2):
                            i_ = e.matmul(po.t[:, :], lhsT=mv.t[:, mc, hm * 128:(hm + 1) * 128], rhs=pTm.t[:, mc, :],
                                          start=(mc == 0), stop=(mc == 1))
                        return i_
                    P.op("pe", fo, reads=[mv, pTm], writes=[po])

                    def fl(e):
                        for mc in range(2):
                            i_ = e.matmul(p2.t[:, :], lhsT=ones.t[:], rhs=pTm.t[:, mc, :], start=(mc == 0),
                                          stop=(mc == 1))
                        return i_
                    P.op("pe", fl, reads=[ones, pTm], writes=[p2])
                    P.op("dve", lambda e: e.reciprocal(out=rf.t[:, :], in_=p2.t[:, :]), reads=[p2], writes=[rf])
                    P.op("dve", lambda e, po=po, hm=hm: e.tensor_tensor(out=yc[hm].t[:, :], in0=po.t[:, :],
                                                                        in1=rf.t[:, :], op=ALU.mult),
                         reads=[po, rf], writes=[yc[hm]])
                rep_rstd([(yc[hm], yc[hm].t[:, :]) for hm in range(4)], 1.0 / 512)
                for hm in range(4):
                    o_ = scaled_out(yc[hm], yc[hm].t[:, :], gout.t[:, 12 + hm:13 + hm], gout)
                    store(o_, YCM[4 + hm, :, oc:oc + 512])
            P.end_phase()
            stop_if(0)

        with ExitStack() as st:
            QTh = mk(st, "QTh", [128, TOWN], BF16)
            KTh = mk(st, "KTh", [128, TLOC], BF16)
            VTh = mk(st, "VTh", [128, TLOC], BF16)
            Vtok = [mk(st, "Vtok%d" % i, [128, 48 * 128], BF16) for i in range(3)]
            accO = mk(st, "accO", [128, TOWN], F32)
            accL = mk(st, "accL", [128, TOWN], F32)
            biasS = mk(st, "biasS_s", [128, 48 * 128], BF16)
            kinv = mk(st, "kinv_s", [1, TLOC], BF16)
            negr = mk(st, "negr", [1, 128], BF16)
            pTs = [mk(st, "pTs%d" % i, [128, 256], BF16) for i in range(2)]
            sqa = mk(st, "sqa", [128, TOWN], BF16)
            psS = [mk(st, "psS%d" % i, [128, 512], F32, psum=True) for i in range(2)]
            psO = [mk(st, "psO%d" % i, [128, 512], F32, psum=True) for i in range(2)]
            psL = [mk(st, "psL%d" % i, [128, 512], F32, psum=True) for i in range(2)]
            pV = mk(st, "pV", [128, 1024], BF16, psum=True)
            psSS = mk(st, "psSS", [128, 512], F32, psum=True)
            P.dma("sp", lambda e: e.dma_start(out=biasS.t[:], in_=biasd), writes=[biasS])
            P.dma("sp", lambda e: e.dma_start(out=kinv.t[:], in_=kinvd), writes=[kinv])
            P.op("pool", lambda e: e.memset(negr.t[:], -1.0e5), writes=[negr])
            nu = 0
            ng = 0
            for h in range(8):
                P.dma("sp", lambda e, h=h: e.dma_start(out=QTh.t[:], in_=QT[h]), writes=[QTh])
                P.dma("sp", lambda e, h=h: e.dma_start(out=KTh.t[:], in_=KT[h]), writes=[KTh])
                P.dma("sp", lambda e, h=h: e.dma_start(out=VTh.t[:], in_=VT[h]), writes=[VTh])
                ev = 0
                for bi, d in enumerate(BRANCH):
                    for i0 in range(0, 48, 8):
                        def ftr(e, i0=i0, d=d):
                            for j in range(8):
                                idx = i0 + j
                                k0 = (idx // d) * 128 * d + (idx % d)
                                i_ = e.transpose(out=pV.t[:, j * 128:(j + 1) * 128], in_=VTh.t[:, ssl(k0, 128, d)],
                                                 identity=ident.t[:])
                            return i_
                        P.op("pe", ftr, reads=[VTh, ident], writes=[pV])
                        if ev % 2 == 0:
                            P.op("act", lambda e, bi=bi, i0=i0: e.activation(
                                out=Vtok[bi].t[:, i0 * 128:(i0 + 8) * 128], in_=pV.t[:, :], func=AF.Copy),
                                reads=[pV], writes=[Vtok[bi]])
                        else:
                            P.op("dve", lambda e, bi=bi, i0=i0: e.tensor_copy(
                                out=Vtok[bi].t[:, i0 * 128:(i0 + 8) * 128], in_=pV.t[:, :]),
                                reads=[pV], writes=[Vtok[bi]])
                        ev += 1
                for bi, d in enumerate(BRANCH):
                    nB0, nB1 = HALO // (128 * d), TLOC // (128 * d)
                    tiles = [(Bq, r) for Bq in range(nB0, nB1) for r in range(d)]
                    for g0 in range(0, len(tiles), 4):
                        grp = tiles[g0:g0 + 4]
                        pO = psO[ng % 2]
                        pL = psL[ng % 2]
                        ng += 1
                        for u, (Bq, r) in enumerate(grp):
                            q0 = Bq * 128 * d + r
                            pS_ = psS[nu % 2]
                            pT_ = pTs[nu % 2]
                            nu += 1

                            def fs(e, q0=q0, d=d, bi=bi, h=h, pS_=pS_):
                                for kt in range(2):
                                    k0 = q0 - (1 - kt) * 128 * d
                                    halo = k0 < HALO
                                    o_ap = pS_.t[:, kt * 128:(kt + 1) * 128]
                                    e.matmul(o_ap, lhsT=KTh.t[:, ssl(k0, 128, d)],
                                             rhs=QTh.t[:, ssl(q0 - HALO, 128, d)], start=True, stop=False)
                                    bc = ((bi * 8 + h) * 2 + kt) * 128
                                    i_ = e.matmul(o_ap, lhsT=ident.t[:], rhs=biasS.t[:, bc:bc + 128], start=False,
                                                  stop=(not halo))
                                    if halo:
                                        i_ = e.matmul(o_ap, lhsT=kinv.t[0:1, ssl(k0, 128, d)], rhs=negr.t[0:1, :],
                                                      start=False, stop=True)
                                return i_
                            P.op("pe", fs, reads=[KTh, QTh, ident, biasS, kinv, negr], writes=[pS_])
                            P.op("act", lambda e, pS_=pS_, pT_=pT_: e.activation(
                                out=pT_.t[:, :], in_=pS_.t[:, 0:256], func=AF.Exp, scale=SC),
                                reads=[pS_], writes=[pT_])

                            def fo(e, q0=q0, d=d, bi=bi, u=u, pT_=pT_, pO=pO, pL=pL):
                                for kt in range(2):
                                    k0 = q0 - (1 - kt) * 128 * d
                                    idx = (k0 // (128 * d)) * d + (k0 % d)
                                    e.matmul(pO.t[:, u * 128:(u + 1) * 128], lhsT=Vtok[bi].t[:, idx * 128:(idx + 1) * 128],
                                             rhs=pT_.t[:, kt * 128:(kt + 1) * 128], start=(kt == 0), stop=(kt == 1))
                                for kt in range(2):
                                    i_ = e.matmul(pL.t[:, u * 128:(u + 1) * 128], lhsT=ones.t[:],
                                                  rhs=pT_.t[:, kt * 128:(kt + 1) * 128], start=(kt == 0),
                                                  stop=(kt == 1))
                                return i_
                            P.op("pe", fo, reads=[Vtok[bi], pT_, ones], writes=[pO, pL])
                        Bq, r0 = grp[0]

                        def view(a):
                            if d == 1:
                                c0 = Bq * 128 - HALO
                                return a.t[:, c0:c0 + 512].rearrange("p (u i) -> p u i", i=128)
                            if d == 4:
                                c0 = Bq * 512 - HALO
                                return a.t[:, c0:c0 + 512].rearrange("p (i u) -> p u i", u=4)
                            c0 = Bq * 2048 - HALO
                            return a.t[:, c0:c0 + 2048].rearrange("p (i r) -> p r i", r=16)[:, r0:r0 + 4, :]
                        vO, vL = view(accO), view(accL)
                        pOv = pO.t[:, :].rearrange("p (u i) -> p u i", i=128)
                        pLv = pL.t[:, :].rearrange("p (u i) -> p u i", i=128)
                        if bi == 0:
                            P.op("act", lambda e, vO=vO, pOv=pOv: e.activation(out=vO, in_=pOv, func=AF.Copy),
                                 reads=[pO], writes=[accO])
                            P.op("dve", lambda e, vL=vL, pLv=pLv: e.tensor_copy(out=vL, in_=pLv),
                                 reads=[pL], writes=[accL])
                        else:
                            P.op("dve", lambda e, vO=vO, pOv=pOv: e.tensor_tensor(out=vO, in0=pOv, in1=vO, op=ALU.add),
                                 reads=[pO, accO], writes=[accO])
                            P.op("dve", lambda e, vL=vL, pLv=pLv: e.tensor_tensor(out=vL, in0=pLv, in1=vL, op=ALU.add),
                                 reads=[pL, accL], writes=[accL])
                P.op("dve", lambda e: e.reciprocal(out=accL.t[:, :], in_=accL.t[:, :]), reads=[accL], writes=[accL])
                P.op("dve", lambda e: e.tensor_tensor(out=accO.t[:, :], in0=accO.t[:, :], in1=accL.t[:, :], op=ALU.mult),
                     reads=[accO, accL], writes=[accO])
                P.op("act", lambda e: e.activation(out=sqa.t[:, :], in_=accO.t[:, :], func=AF.Square),
                     reads=[accO], writes=[sqa])

                def fss(e):
                    for t_ in range(NT):
                        i_ = e.matmul(psSS.t[:, t_:t_ + 1], lhsT=sqa.t[:, t_ * 128:(t_ + 1) * 128], rhs=ones.t[:, 0:1],
                                      start=True, stop=True)
                    return i_
                P.op("pe", fss, reads=[sqa, ones], writes=[psSS])
                if h == 0:
                    P.op("dve", lambda e: e.tensor_copy(out=ssA.t[:, :], in_=psSS.t[:, 0:NT]), reads=[psSS], writes=[ssA])
                else:
                    P.op("dve", lambda e: e.tensor_tensor(out=ssA.t[:, :], in0=psSS.t[:, 0:NT], in1=ssA.t[:, :],
                                                          op=ALU.add), reads=[psSS, ssA], writes=[ssA])
                P.op("act", lambda e, h=h: e.activation(out=sqa.t[:, :], in_=accO.t[:, :], func=AF.Copy,
                                                        scale=gout.t[:, h:h + 1]), reads=[accO, gout], writes=[sqa])
                P.dma("sp", lambda e, h=h: e.dma_start(out=ATT[h], in_=sqa.t[:, :]), reads=[sqa], writes=[TB()])
            P.end_phase()
            stop_if(1)

        with ExitStack() as st2:
            A0 = mk(st2, "A0", [128, NT, 64], BF16)
            A1 = mk(st2, "A1", [128, NT, 64], BF16)
            Ab = mk(st2, "Ab", [128, NT, 64], BF16)
            with ExitStack() as st:
                Wo = mk(st, "Wo", [128, 16, D], BF16)
                Wr = mk(st, "Wr", [128, 16, 72], BF16)
                gbc = mk(st, "gbc2", [128, D], F32)
                brb = mk(st, "brb", [128, 72], F32)
                rstdA = mk(st, "rstdA", [128, NT], F32)
                tmpA = mk(st, "tmpA", [128, NT], F32)
                aT = [mk(st, "aT%d" % i, [128, 8, 128], BF16) for i in range(2)]
                yT = [mk(st, "yT%d" % i, [128, 8, 128], BF16) for i in range(2)]
                xt = [mk(st, "xt2_%d" % i, [128, D], F32) for i in range(2)]
                x1 = [mk(st, "x1_%d" % i, [128, D], F32) for i in range(2)]
                junk = mk(st, "junk2", [128, D], BF16)
                ss = mk(st, "ss2", [128, 1], F32)
                tmp1 = mk(st, "tmp2", [128, 1], F32)
                rstd = mk(st, "rstd2", [128, 1], F32)
                h2 = [mk(st, "h2_%d" % i, [128, D], BF16) for i in range(2)]
                h2T = mk(st, "h2T", [128, 16, 128], BF16)
                lg = mk(st, "lg", [128, 72], F32)
                m8 = mk(st, "m8", [128, 8], F32)
                m8e = mk(st, "m8e", [128, 8], F32)
                ohg = mk(st, "ohg", [128, 8], F32)
                oh = [mk(st, "oh%d" % i, [128, 8], F32) for i in range(2)]
                le = mk(st, "le", [128, 8], F32)
                sm = mk(st, "sm", [128, 8], F32)
                pA = [mk(st, "pA%d" % i, [128, 512], F32, psum=True) for i in range(2)]
                pB = [mk(st, "pB%d" % i, [128, 512], F32, psum=True) for i in range(2)]
                pT = mk(st, "pT3", [128, 2048], BF16, psum=True)
                pR = mk(st, "pR", [128, 512], F32, psum=True)
                cast_load_w(Wo, w_out, D, 0, 0)
                for k in range(16):
                    P.dma("pool", lambda e, k=k: e.dma_start(out=Wr.t[:, k, :], in_=w_r[k * 128:(k + 1) * 128, :]),
                          writes=[Wr])
                P.dma("sp", lambda e: e.dma_start(out=gbc.t[:], in_=g2bc), writes=[gbc])
                P.dma("sp", lambda e: e.dma_start(out=brb.t[:], in_=brbc), writes=[brb])
                rstd_of(ssA.t[:, :], ssA, tmpA, tmpA.t[:, :], rstdA, rstdA.t[:, :], 1.0 / 1024)
                ATTv = ATT.rearrange("c p n -> p c n")
                YCMv = YCM.rearrange("c p n -> p c n")
                npp = 0
                for t_ in range(NT):
                    a_, y_, x_, x1_, h2_ = aT[t_ % 2], yT[t_ % 2], xt[t_ % 2], x1[t_ % 2], h2[t_ % 2]
                    c0 = t_ * 128
                    P.dma("sp", lambda e, a_=a_, c0=c0: e.dma_start(out=a_.t[:], in_=ATTv[:, :, c0:c0 + 128]), writes=[a_])
                    P.dma("sp", lambda e, y_=y_, c0=c0: e.dma_start(out=y_.t[:], in_=YCMv[:, :, c0:c0 + 128]), writes=[y_])
                    P.dma("sp", lambda e, x_=x_, c0=c0: e.dma_start(out=x_.t[:], in_=xh[HALO + c0:HALO + c0 + 128, :]),
                          writes=[x_])
                    for nb in range(4):
                        pa, pb_ = pA[npp % 2], pB[npp % 2]
                        npp += 1

                        def fa(e, pa=pa, a_=a_, nb=nb):
                            for c in range(8):
                                i_ = e.matmul(pa.t[:, :], lhsT=a_.t[:, c, :], rhs=Wo.t[:, c, nb * 512:(nb + 1) * 512],
                                              start=(c == 0), stop=(c == 7))
                            return i_
                        P.op("pe", fa, reads=[a_, Wo], writes=[pa])

                        def fb(e, pb_=pb_, y_=y_, nb=nb):
                            for c in range(8):
                                i_ = e.matmul(pb_.t[:, :], lhsT=y_.t[:, c, :], rhs=Wo.t[:, 8 + c, nb * 512:(nb + 1) * 512],
                                              start=(c == 0), stop=(c == 7))
                            return i_
                        P.op("pe", fb, reads=[y_, Wo], writes=[pb_])
                        P.op("dve", lambda e, pa=pa, x_=x_, x1_=x1_, nb=nb, t_=t_: e.scalar_tensor_tensor(
                            out=x1_.t[:, nb * 512:(nb + 1) * 512], in0=pa.t[:, :], scalar=rstdA.t[:, t_:t_ + 1],
                            in1=x_.t[:, nb * 512:(nb + 1) * 512], op0=ALU.mult, op1=ALU.add),
                            reads=[pa, rstdA, x_], writes=[x1_])
                        P.op("dve", lambda e, pb_=pb_, x1_=x1_, nb=nb: e.tensor_tensor(
                            out=x1_.t[:, nb * 512:(nb + 1) * 512], in0=pb_.t[:, :], in1=x1_.t[:, nb * 512:(nb + 1) * 512],
                            op=ALU.add), reads=[pb_, x1_], writes=[x1_])
                    P.dma("sp", lambda e, x1_=x1_, c0=c0: e.dma_start(out=X1[c0:c0 + 128, :], in_=x1_.t[:]),
                          reads=[x1_], writes=[TB()])
                    P.op("act", lambda e, x1_=x1_: e.activation(out=junk.t[:], in_=x1_.t[:], func=AF.Square,
                                                                accum_out=ss.t[:]), reads=[x1_], writes=[junk, ss])
                    rstd_of(ss.t[:], ss, tmp1, tmp1.t[:], rstd, rstd.t[:], 1.0 / D)
                    P.op("dve", lambda e, x1_=x1_, h2_=h2_: e.scalar_tensor_tensor(
                        out=h2_.t[:], in0=x1_.t[:], scalar=rstd.t[:, 0:1], in1=gbc.t[:], op0=ALU.mult, op1=ALU.mult),
                        reads=[x1_, rstd, gbc], writes=[h2_])
                    P.dma("sp", lambda e, h2_=h2_, c0=c0: e.dma_start(out=H2[c0:c0 + 128, :], in_=h2_.t[:]),
                          reads=[h2_], writes=[TB()])

                    def tr(e, h2_=h2_):
                        for k in range(16):
                            i_ = e.transpose(out=pT.t[:, k * 128:(k + 1) * 128], in_=h2_.t[:, k * 128:(k + 1) * 128],
                                             identity=ident.t[:])
                        return i_
                    P.op("pe", tr, reads=[h2_, ident], writes=[pT])
                    P.op("act", lambda e: e.activation(out=h2T.t[:, 0:8, :],
                                                       in_=pT.t[:, 0:1024].rearrange("p (k n) -> p k n", n=128),
                                                       func=AF.Copy), reads=[pT], writes=[h2T])
                    P.op("dve", lambda e: e.tensor_copy(out=h2T.t[:, 8:16, :],
                                                        in_=pT.t[:, 1024:2048].rearrange("p (k n) -> p k n", n=128)),
                         reads=[pT], writes=[h2T])

                    def fr(e):
                        for k in range(16):
                            i_ = e.matmul(pR.t[:, 0:72], lhsT=h2T.t[:, k, :], rhs=Wr.t[:, k, :], start=(k == 0),
                                          stop=(k == 15))
                        return i_
                    P.op("pe", fr, reads=[h2T, Wr], writes=[pR])
                    P.op("dve", lambda e: e.tensor_tensor(out=lg.t[:, :], in0=pR.t[:, 0:72], in1=brb.t[:, :], op=ALU.add),
                         reads=[pR, brb], writes=[lg])
                    P.op("dve", lambda e: e.max(out=m8.t[:, :], in_=lg.t[:, 0:8]), reads=[lg], writes=[m8])
                    P.op("dve", lambda e: e.tensor_scalar(out=ohg.t[:, :], in0=lg.t[:, 0:8], scalar1=m8.t[:, 0:1],
                                                          scalar2=None, op0=ALU.is_equal), reads=[lg, m8], writes=[ohg])
                    P.op("dve", lambda e: e.tensor_scalar(out=sm.t[:, 0:1], in0=m8.t[:, 0:1], scalar1=-1.0, scalar2=None,
                                                          op0=ALU.mult), reads=[m8], writes=[sm])
                    P.op("act", lambda e: e.activation(out=le.t[:, :], in_=lg.t[:, 0:8], func=AF.Exp, bias=sm.t[:, 0:1],
                                                       scale=1.0, accum_out=sm.t[:, 1:2]), reads=[lg, sm], writes=[le, sm])
                    P.op("dve", lambda e: e.reciprocal(out=sm.t[:, 2:3], in_=sm.t[:, 1:2]), reads=[sm], writes=[sm])
                    for g in range(8):
                        if g == 0:
                            P.op("dve", lambda e: e.tensor_scalar(out=le.t[:, :], in0=lg.t[:, 8:16], scalar1=ohg.t[:, 0:1],
                                                                  scalar2=None, op0=ALU.mult), reads=[lg, ohg], writes=[le])
                        else:
                            P.op("dve", lambda e, g=g: e.scalar_tensor_tensor(
                                out=le.t[:, :], in0=lg.t[:, 8 + g * 8:16 + g * 8], scalar=ohg.t[:, g:g + 1], in1=le.t[:, :],
                                op0=ALU.mult, op1=ALU.add), reads=[lg, ohg, le], writes=[le])
                    P.op("dve", lambda e: e.max(out=m8e.t[:, :], in_=le.t[:, :]), reads=[le], writes=[m8e])
                    for k_ in range(2):
                        P.op("dve", lambda e, k_=k_: e.tensor_scalar(out=oh[k_].t[:, :], in0=le.t[:, :],
                                                                     scalar1=m8e.t[:, k_:k_ + 1], scalar2=None,
                                                                     op0=ALU.is_equal), reads=[le, m8e], writes=[oh[k_]])
                    P.op("dve", lambda e: e.tensor_tensor(out=sm.t[:, 3:4], in0=m8e.t[:, 1:2], in1=m8e.t[:, 0:1],
                                                          op=ALU.subtract), reads=[m8e], writes=[sm])
                    P.op("act", lambda e: e.activation(out=sm.t[:, 4:5], in_=sm.t[:, 3:4], func=AF.Exp),
                         reads=[sm], writes=[sm])
                    P.op("dve", lambda e: e.tensor_scalar(out=sm.t[:, 5:6], in0=sm.t[:, 4:5], scalar1=1.0, scalar2=None,
                                                          op0=ALU.add), reads=[sm], writes=[sm])
                    P.op("dve", lambda e: e.reciprocal(out=sm.t[:, 6:7], in_=sm.t[:, 5:6]), reads=[sm], writes=[sm])
                    P.op("dve", lambda e, t_=t_: e.tensor_tensor(out=gates.t[:, t_, 0:1], in0=sm.t[:, 6:7], in1=sm.t[:, 2:3],
                                                                 op=ALU.mult), reads=[sm], writes=[gates])
                    P.op("dve", lambda e, t_=t_: e.tensor_tensor(out=gates.t[:, t_, 1:2], in0=sm.t[:, 2:3],
                                                                 in1=gates.t[:, t_, 0:1], op=ALU.subtract),
                         reads=[sm, gates], writes=[gates])
                    for k_, A_ in enumerate((A0, A1)):
                        for g in range(8):
                            P.op("dve", lambda e, k_=k_, A_=A_, g=g, t_=t_: e.tensor_scalar(
                                out=A_.t[:, t_, g * 8:(g + 1) * 8], in0=oh[k_].t[:, :], scalar1=ohg.t[:, g:g + 1],
                                scalar2=None, op0=ALU.mult), reads=[oh[k_], ohg], writes=[A_])
                    P.op("dve", lambda e, t_=t_: e.tensor_tensor(out=Ab.t[:, t_, :], in0=A0.t[:, t_, :], in1=A1.t[:, t_, :],
                                                                 op=ALU.add), reads=[A0, A1], writes=[Ab])
                P.end_phase()
                stop_if(2)

            with ExitStack() as st:
                triu = mk(st, "triu_s", [128, 128], BF16)
                base8 = mk(st, "base8_s", [128, 8], F32)
                cntf = mk(st, "cntf", [128, 64], F32)
                ci = mk(st, "ci", [128, 64], I32)
                padf = mk(st, "padf", [128, 64], F32)
                zer = mk(st, "zer", [128, 64], F32)
                pends = mk(st, "pends", [128, 64], F32)
                pstart = mk(st, "pstart", [128, 64], F32)
                jk = mk(st, "jk", [128, 64], F32)
                be = mk(st, "be", [128, NBLK], F32)
                idxf = mk(st, "idxf", [128, NBLK, 8], F32)
                Acum = mk(st, "Acum", [128, 64], BF16)
                basef = mk(st, "basef", [128, 64], F32)
                dstf = mk(st, "dstf", [128, 2], F32)
                h2t = [mk(st, "h2t%d" % i, [128, D], BF16) for i in range(2)]
                pC = mk(st, "pC", [128, 512], F32, psum=True)
                pK = [mk(st, "pK%d" % i, [128, 512], F32, psum=True) for i in range(2)]
                P.dma("sp", lambda e: e.dma_start(out=triu.t[:], in_=triud), writes=[triu])
                P.dma("sp", lambda e: e.dma_start(out=base8.t[:], in_=base8d), writes=[base8])
                P.op("pool", lambda e: e.memset(zer.t[:], 0.0), writes=[zer])
                P.op("pool", lambda e: e.memset(Acum.t[:], 0.0), writes=[Acum])

                def fc(e):
                    for t_ in range(NT):
                        i_ = e.matmul(pC.t[:, 0:64], lhsT=ones.t[:], rhs=Ab.t[:, t_, :], start=(t_ == 0),
                                      stop=(t_ == NT - 1))
                    return i_
                P.op("pe", fc, reads=[ones, Ab], writes=[pC])
                P.op("dve", lambda e: e.tensor_scalar(out=ci.t[:, :], in0=pC.t[:, 0:64], scalar1=127.0, scalar2=None,
                                                      op0=ALU.add), reads=[pC], writes=[ci])
                P.op("dve", lambda e: e.tensor_single_scalar(out=ci.t[:, :], in_=ci.t[:, :], scalar=-128,
                                                             op=ALU.bitwise_and), reads=[ci], writes=[ci])
                P.op("dve", lambda e: e.tensor_copy(out=padf.t[:, :], in_=ci.t[:, :]), reads=[ci], writes=[padf])
                P.op("dve", lambda e: e.tensor_tensor_scan(out=pends.t[:, :], data0=padf.t[:, :], data1=zer.t[:, :],
                                                           initial=0.0, op0=ALU.add, op1=ALU.add),
                     reads=[padf, zer], writes=[pends])
                P.op("dve", lambda e: e.tensor_tensor(out=pstart.t[:, :], in0=pends.t[:, :], in1=padf.t[:, :],
                                                      op=ALU.subtract), reads=[pends, padf], writes=[pstart])
                for b in range(NBLK):
                    P.op("dve", lambda e, b=b: e.tensor_scalar(out=jk.t[:, :], in0=pends.t[:, :], scalar1=float(128 * b),
                                                               scalar2=None, op0=ALU.is_le, op1=ALU.add,
                                                               accum_out=be.t[:, b:b + 1]), reads=[pends], writes=[jk, be])
                P.op("dve", lambda e: e.tensor_scalar(out=be.t[:, :], in0=be.t[:, :], scalar1=63.0, scalar2=256.0,
                                                      op0=ALU.min, op1=ALU.mult), reads=[be], writes=[be])
                for b in range(NBLK):
                    P.op("dve", lambda e, b=b: e.tensor_scalar(out=idxf.t[:, b, :], in0=base8.t[:, :],
                                                               scalar1=be.t[:, b:b + 1], scalar2=None, op0=ALU.add),
                         reads=[base8, be], writes=[idxf])
                P.op("dve", lambda e: e.tensor_copy(out=idxW.t[:, :, :], in_=idxf.t[:, :, :]), reads=[idxf], writes=[idxW])
                for t_ in range(NT):
                    pk = pK[t_ % 2]
                    h_ = h2t[t_ % 2]
                    P.dma("sp", lambda e, h_=h_, t_=t_: e.dma_start(out=h_.t[:], in_=H2[t_ * 128:(t_ + 1) * 128, :]),
                          writes=[h_])

                    def fk(e, pk=pk, t_=t_):
                        e.matmul(pk.t[:, 0:64], lhsT=triu.t[:], rhs=Ab.t[:, t_, :], start=True, stop=False)
                        return e.matmul(pk.t[:, 0:64], lhsT=ones.t[:], rhs=Acum.t[:, :], start=False, stop=True)
                    P.op("pe", fk, reads=[triu, ones, Ab, Acum], writes=[pk])
                    P.op("dve", lambda e, t_=t_: e.tensor_tensor(out=Acum.t[:, :], in0=Acum.t[:, :], in1=Ab.t[:, t_, :],
                                                                 op=ALU.add), reads=[Acum, Ab], writes=[Acum])
                    P.op("dve", lambda e, pk=pk: e.tensor_tensor(out=basef.t[:, :], in0=pk.t[:, 0:64], in1=pstart.t[:, :],
                                                                 op=ALU.add), reads=[pk, pstart], writes=[basef])
                    for k_, A_ in enumerate((A0, A1)):
                        P.op("dve", lambda e, A_=A_, t_=t_: e.tensor_tensor(out=jk.t[:, :], in0=basef.t[:, :],
                                                                            in1=A_.t[:, t_, :], op=ALU.mult),
                             reads=[basef, A_], writes=[jk])
                        P.op("dve", lambda e, k_=k_: e.tensor_reduce(out=dstf.t[:, k_:k_ + 1], in_=jk.t[:, :], axis=AX.X,
                                                                     op=ALU.add), reads=[jk], writes=[dstf])
                    P.op("dve", lambda e, t_=t_: e.tensor_copy(out=desti.t[:, t_, :], in_=dstf.t[:, :]),
                         reads=[dstf], writes=[desti])
                    for k_ in range(2):
                        P.dma("pool", lambda e, h_=h_, t_=t_, k_=k_: e.indirect_dma_start(
                            out=XBUF[:, :], out_offset=bass.IndirectOffsetOnAxis(ap=desti.t[:, t_, k_:k_ + 1], axis=0),
                            in_=h_.t[:, :], in_offset=None), reads=[h_, desti], writes=[TB()])
                P.end_phase()
                stop_if(3)

        with ExitStack() as st:
            NSLOT = 4
            slot = [mk(st, "slot%d" % i, [128, 8, 2048], BF16) for i in range(NSLOT)]
            xb = [mk(st, "xb%d" % i, [128, D], BF16) for i in range(2)]
            xbT = mk(st, "xbT", [128, 16, 128], BF16)
            sil = mk(st, "sil", [128, 512], F32)
            act = mk(st, "act", [128, 1024], BF16)
            actT = mk(st, "actT", [128, 8, 128], BF16)
            yb = [mk(st, "yb%d" % i, [128, D], F32) for i in range(2)]
            dbg_out.update(xb0=xb[0], xb1=xb[1], yb0=yb[0], yb1=yb[1])
            pT = mk(st, "pT4", [128, 2048], BF16, psum=True)
            pG = [mk(st, "pG%d" % i, [128, 512], F32, psum=True) for i in range(2)]
            pU = [mk(st, "pU%d" % i, [128, 512], F32, psum=True) for i in range(2)]
            pY = [mk(st, "pY%d" % i, [128, 512], F32, psum=True) for i in range(2)]
            ns = 0
            ng = 0
            ny = 0
            for b in range(_DBG.get("nblk", NBLK)):
                xb_ = xb[b % 2]
                P.dma("sp", lambda e, xb_=xb_, b=b: e.dma_start(out=xb_.t[:], in_=XBUF[b * 128:(b + 1) * 128, :]),
                      writes=[xb_])
                ws = []
                for wsrc in (wg, wu, wd):
                    s_ = slot[ns % NSLOT]
                    ns += 1
                    if "w" in _DBG.get("skip", ()):
                        P.op("pool", lambda e, s_=s_: e.memset(s_.t[:], 0.01), writes=[s_])
                    for j in range(0 if "w" in _DBG.get("skip", ()) else 8):
                        P.dma("pool", lambda e, s_=s_, j=j, b=b, wsrc=wsrc: e.indirect_dma_start(
                            out=s_.t[:, j, :], out_offset=None, in_=wsrc[j % 4][:, :],
                            in_offset=bass.IndirectOffsetOnAxis(ap=idxW.t[:, b, j:j + 1], axis=0)),
                            reads=[idxW], writes=[s_])
                    ws.append(s_)
                sg, su, sd = ws

                def tr(e, xb_=xb_):
                    for k in range(16):
                        i_ = e.transpose(out=pT.t[:, k * 128:(k + 1) * 128], in_=xb_.t[:, ssl(k, 128, 16)],
                                         identity=ident.t[:])
                    return i_
                P.op("pe", tr, reads=[xb_, ident], writes=[pT])
                P.op("act", lambda e: e.activation(out=xbT.t[:, 0:8, :],
                                                   in_=pT.t[:, 0:1024].rearrange("p (k n) -> p k n", n=128), func=AF.Copy),
                     reads=[pT], writes=[xbT])
                P.op("dve", lambda e: e.tensor_copy(out=xbT.t[:, 8:16, :],
                                                    in_=pT.t[:, 1024:2048].rearrange("p (k n) -> p k n", n=128)),
                     reads=[pT], writes=[xbT])
                for hf in range(2):
                    pg, pu = pG[ng % 2], pU[ng % 2]
                    ng += 1
                    for (pp_, sw) in ((pg, sg), (pu, su)):
                        def fm(e, pp_=pp_, sw=sw, hf=hf):
                            for k in range(16):
                                c0 = (k % 2) * 1024 + hf * 512
                                i_ = e.matmul(pp_.t[:, :], lhsT=xbT.t[:, k, :], rhs=sw.t[:, k // 2, c0:c0 + 512],
                                              start=(k == 0), stop=(k == 15))
                            return i_
                        P.op("pe", fm, reads=[xbT, sw], writes=[pp_])
                    P.op("act", lambda e, pg=pg: e.activation(out=sil.t[:, :], in_=pg.t[:, :], func=(AF.Copy if "silu" in _DBG.get("skip", ()) else AF.Silu)),
                         reads=[pg], writes=[sil])
                    P.op("dve", lambda e, pu=pu, hf=hf: e.tensor_tensor(out=act.t[:, hf * 512:(hf + 1) * 512],
                                                                        in0=sil.t[:, :], in1=pu.t[:, :], op=ALU.mult),
                         reads=[sil, pu], writes=[act])

                def tr2(e):
                    for k in range(8):
                        i_ = e.transpose(out=pT.t[:, k * 128:(k + 1) * 128], in_=act.t[:, ssl(k, 128, 8)],
                                         identity=ident.t[:])
                    return i_
                P.op("pe", tr2, reads=[act, ident], writes=[pT])
                P.op("act", lambda e: e.activation(out=actT.t[:, :, :],
                                                   in_=pT.t[:, 0:1024].rearrange("p (k n) -> p k n", n=128), func=AF.Copy),
                     reads=[pT], writes=[actT])
                yb_ = yb[b % 2]
                for nb in range(4):
                    py = pY[ny % 2]
                    ny += 1

                    def fd(e, py=py, sd=sd, nb=nb):
                        for k in range(8):
                            i_ = e.matmul(py.t[:, :], lhsT=actT.t[:, k, :], rhs=sd.t[:, k, nb * 512:(nb + 1) * 512],
                                          start=(k == 0), stop=(k == 7))
                        return i_
                    P.op("pe", fd, reads=[actT, sd], writes=[py])
                    if nb % 2 == 0:
                        P.op("act", lambda e, py=py, yb_=yb_, nb=nb: e.activation(
                            out=yb_.t[:, nb * 512:(nb + 1) * 512], in_=py.t[:, :], func=AF.Copy), reads=[py], writes=[yb_])
                    else:
                        P.op("dve", lambda e, py=py, yb_=yb_, nb=nb: e.tensor_copy(
                            out=yb_.t[:, nb * 512:(nb + 1) * 512], in_=py.t[:, :]), reads=[py], writes=[yb_])
                P.dma("sp", lambda e, yb_=yb_, b=b: e.dma_start(out=YBUF[b * 128:(b + 1) * 128, :], in_=yb_.t[:]),
                      reads=[yb_], writes=[TB()])
            P.end_phase()
            stop_if(4)

        with ExitStack() as st:
            y0 = [mk(st, "y0_%d" % i, [128, D], F32) for i in range(2)]
            y1 = [mk(st, "y1_%d" % i, [128, D], F32) for i in range(2)]
            xo = [mk(st, "xo_%d" % i, [128, D], F32) for i in range(2)]
            for t_ in range(NT):
                a_, b_, x_ = y0[t_ % 2], y1[t_ % 2], xo[t_ % 2]
                P.dma("sp", lambda e, x_=x_, t_=t_: e.dma_start(out=x_.t[:], in_=X1[t_ * 128:(t_ + 1) * 128, :]), writes=[x_])
                for k_, y_ in enumerate((a_, b_)):
                    P.dma("pool", lambda e, y_=y_, t_=t_, k_=k_: e.indirect_dma_start(
                        out=y_.t[:, :], out_offset=None, in_=YBUF[:, :],
                        in_offset=bass.IndirectOffsetOnAxis(ap=desti.t[:, t_, k_:k_ + 1], axis=0)),
                        reads=[desti], writes=[y_])
                P.op("dve", lambda e, a_=a_, x_=x_, t_=t_: e.scalar_tensor_tensor(
                    out=x_.t[:], in0=a_.t[:], scalar=gates.t[:, t_, 0:1], in1=x_.t[:], op0=ALU.mult, op1=ALU.add),
                    reads=[a_, gates, x_], writes=[x_])
                P.op("dve", lambda e, b_=b_, x_=x_, t_=t_: e.scalar_tensor_tensor(
                    out=x_.t[:], in0=b_.t[:], scalar=gates.t[:, t_, 1:2], in1=x_.t[:], op0=ALU.mult, op1=ALU.add),
                    reads=[b_, gates, x_], writes=[x_])
                P.dma("sp", lambda e, x_=x_, t_=t_: e.dma_start(out=out[t_ * 128:(t_ + 1) * 128, :], in_=x_.t[:]),
                      reads=[x_], writes=[TB()])
            P.end_phase()
            stop_if(5)
    except _Stop:
        pass
    return nc


def _consts():
    ident = np.eye(128, dtype=np.float32).astype(BF)
    p = np.arange(128)
    triu = (p[:, None] < p[None, :]).astype(np.float32).astype(BF)
    slopes = 2.0 ** (-8.0 * np.arange(1, 9) / 8.0)
    bias = np.zeros((128, 48, 128), np.float32)
    j = p[:, None]
    i = p[None, :]
    for bi, d in enumerate(BRANCH):
        for h in range(8):
            for kt in range(2):
                dist = i + 128 - j if kt == 0 else i - j
                valid = (dist >= 0) & (dist <= 128)
                b = -slopes[h] * d * dist * (128.0 ** 0.5)
                bias[:, (bi * 8 + h) * 2 + kt, :] = np.where(valid, b, -1.0e5)
    base8 = (p[:, None] * 2 + (np.arange(8) // 4)[None, :]).astype(np.float32)
    return ident, triu, bias.reshape(128, 48 * 128).astype(BF), base8


def _cols(v, n):
    return np.ascontiguousarray(np.asarray(v, np.float32).reshape(n, 128).T)


_NC_CACHE = {}
_DBG = {}


def kernel(x, mem, norm1_g, w_in, q_norm_g, k_norm_g, conv_w, mem_norm_g, w_mem_kv, mem_q_norm_g, mem_k_norm_g,
           out_norm_g, w_out, norm2_g, w_router_group, b_router_group, w_router_expert, b_router_expert,
           w_gate, w_up, w_down):
    f = lambda a: np.asarray(a, np.float32)
    x = f(x)
    mem = f(mem)
    ident, triu, biasS, base8 = _consts()
    rep = lambda v: np.ascontiguousarray(np.broadcast_to(f(v).reshape(1, -1), (128, f(v).size)))
    shared = {
        "g1bc": rep(norm1_g[0]), "gmbc": rep(mem_norm_g[0]), "g2bc": rep(norm2_g[0]),
        "brbc": rep(np.concatenate([f(b_router_group[0]), f(b_router_expert[0])])),
        "gcols": np.ascontiguousarray(np.stack([f(q_norm_g[0]), f(k_norm_g[0]), f(mem_q_norm_g[0]),
                                                f(mem_k_norm_g[0])], axis=1)),
        "goutc": _cols(out_norm_g[0], 16),
        "convc": np.ascontiguousarray(f(conv_w[0]).reshape(3, 4, 128).transpose(2, 1, 0).reshape(128, 12)),
        "w_in": np.ascontiguousarray(f(w_in[0])), "w_mem": np.ascontiguousarray(f(w_mem_kv[0])),
        "w_out": np.ascontiguousarray(f(w_out[0])),
        "w_r": np.ascontiguousarray(np.concatenate([f(w_router_group[0]), f(w_router_expert[0])], axis=1)),
        "ident": ident, "triu": triu, "biasS": biasS, "base8": base8,
    }
    for nm, w_ in (("wg", w_gate), ("wu", w_up), ("wd", w_down)):
        w4 = f(w_[0]).reshape(16384, 4, 2048)
        for q in range(4):
            shared["%s%d" % (nm, q)] = np.ascontiguousarray(w4[:, q, :])
    in_maps = []
    for c in range(NCORE):
        b, s0 = c // 4, (c % 4) * TOWN
        xhc = np.zeros((TLOC, D), np.float32)
        if s0 > 0:
            xhc[:HALO] = x[b, s0 - HALO:s0]
        xhc[HALO:] = x[b, s0:s0 + TOWN]
        kinv = np.zeros((1, TLOC), np.float32)
        if s0 == 0:
            kinv[0, :HALO] = 1.0
        m = dict(shared)
        m["xh"] = xhc
        m["memx"] = np.ascontiguousarray(mem[b])
        m["kinv"] = kinv.astype(BF)
        in_maps.append(m)
    if _DBG.get("upto"):
        lv = ["A1", "A2", "A3", "M2", "M3", "M4"].index(_DBG["upto"])
        if lv < 4:
            for m in in_maps:
                for k_ in [a_ + str(q_) for a_ in ("wg", "wu", "wd") for q_ in range(4)]:
                    m.pop(k_)
        nc = build_nc(_DBG["upto"], debug=_DBG["expose"])
        res = run_bass_kernel_spmd(nc, in_maps, core_ids=list(range(NCORE)))
        _DBG["res"] = res.results
        _DBG["in_maps"] = in_maps
        return None
    if "nc" not in _NC_CACHE:
        _NC_CACHE["nc"] = build_nc()
    res = run_bass_kernel_spmd(_NC_CACHE["nc"], in_maps, core_ids=list(range(NCORE)))
    outp = np.empty((2, 16384, D), np.float32)
    for c in range(NCORE):
        b, s0 = c // 4, (c % 4) * TOWN
        outp[b, s0:s0 + TOWN] = res.results[c]["out"]
    return outp
```

```python
import numpy as np
import ml_dtypes
from contextlib import ExitStack
import concourse.bass as bass
import concourse.mybir as mybir
from concourse.bass_utils import run_bass_kernel_spmd

F32 = mybir.dt.float32
BF16 = mybir.dt.bfloat16
I32 = mybir.dt.int32
AF = mybir.ActivationFunctionType
ALU = mybir.AluOpType
AX = mybir.AxisListType
BF = ml_dtypes.bfloat16

NCORE = 8
D = 2048
TOWN = 4096
HALO = 2048
TLOC = TOWN + HALO
NT = TOWN // 128
EPS = 1e-6
NBLK = 128
RBUF = NBLK * 128
SC = 128.0 ** -0.5
BRANCH = (1, 4, 16)


def ssl(start, count, step=1):
    return slice(start, start + step * (count - 1) + 1, step)


class _Stop(Exception):
    pass


class TB:
    __slots__ = ("w", "r")

    def __init__(self):
        self.w = None
        self.r = {}


class T:
    def __init__(self, t):
        self.t = t
        self.b = TB()


class Prog:
    ENG = ("pe", "act", "dve", "pool", "sp")

    def __init__(self, nc, stack, ndma=24, same_engine_sync=True):
        self.nc = nc
        self.ndma = ndma
        self.same = same_engine_sync
        self.sems = {}
        for e in ("pe", "act", "dve", "pool"):
            self.sems[e] = stack.enter_context(nc.semaphore("c_" + e))
        for i in range(ndma):
            self.sems[("d", i)] = stack.enter_context(nc.semaphore("d%d" % i))
        self.cnt = {e: 0 for e in ("pe", "act", "dve", "pool")}
        self.dcnt = [0] * ndma
        self.rr = 0
        self.known = {e: {} for e in self.ENG}
        self.streams = {e: [] for e in self.ENG}

    def _deps(self, eng, reads, writes, extra=()):
        need = {}

        def add(tok):
            k, v = tok
            if k == "pe" and eng == "pe":
                return
            if k == eng and not self.same:
                return
            if self.known[eng].get(k, 0) >= v:
                return
            if need.get(k, 0) < v:
                need[k] = v

        for b in reads:
            if b.w is not None:
                add(b.w)
        for b in writes:
            if b.w is not None:
                add(b.w)
            for k, v in b.r.items():
                add((k, v))
        for t in extra:
            add(t)
        for k, v in need.items():
            self.known[eng][k] = v
            self.streams[eng].append(("wait", k, v))

    def _commit(self, tok, reads, writes):
        k, v = tok
        for b in reads:
            if b.r.get(k, 0) < v:
                b.r[k] = v
        for b in writes:
            b.w = tok
            b.r = {}

    def op(self, eng, fn, reads=(), writes=()):
        reads = [x.b if isinstance(x, T) else x for x in reads]
        writes = [x.b if isinstance(x, T) else x for x in writes]
        self._deps(eng, reads, writes)
        self.cnt[eng] += 1
        tok = (eng, self.cnt[eng])
        self.streams[eng].append(("op", fn, eng))
        self._commit(tok, reads, writes)
        return tok

    def dma(self, q, fn, reads=(), writes=()):
        reads = [x.b if isinstance(x, T) else x for x in reads]
        writes = [x.b if isinstance(x, T) else x for x in writes]
        s = self.rr
        self.rr = (s + 1) % self.ndma
        extra = []
        if self.dcnt[s] > 0:
            extra.append((("d", s), self.dcnt[s]))
        self._deps(q, reads, writes, extra)
        self.dcnt[s] += 16
        tok = (("d", s), self.dcnt[s])
        self.streams[q].append(("dma", fn, ("d", s)))
        self._commit(tok, reads, writes)
        return tok

    def reset_sems(self):
        sems = list(self.sems.values())
        with self.nc.Block() as block:
            def f(e):
                for s_ in sems:
                    e.sem_clear(s_)
            block.gpsimd(f)

    def end_phase(self):
        for e in ("sp", "pool"):
            for s in range(self.ndma):
                if self.dcnt[s] > 0 and self.known[e].get(("d", s), 0) < self.dcnt[s]:
                    self.known[e][("d", s)] = self.dcnt[s]
                    self.streams[e].append(("wait", ("d", s), self.dcnt[s]))
        self.flush()

    def flush(self):
        nc = self.nc
        engobj = {"pe": "tensor", "act": "scalar", "dve": "vector", "pool": "gpsimd", "sp": "sync"}
        sems = self.sems

        def run(e, lst):
            for it in lst:
                if it[0] == "wait":
                    e.wait_ge(sems[it[1]], it[2])
                elif it[0] == "op":
                    it[1](e).then_inc(sems[it[2]], 1)
                else:
                    it[1](e).then_inc(sems[it[2]], 16)

        with nc.Block() as block:
            for en in self.ENG:
                lst = self.streams[en]
                if lst:
                    getattr(block, engobj[en])(lambda e, lst=lst: run(e, lst))
        self.streams = {e: [] for e in self.ENG}


def build_nc(upto="M4", debug=False):
    nc = bass.Bass("TRN2", target_bir_lowering=False)
    PH = ["A1", "A2", "A3", "M2", "M3", "M4"]
    lvl = PH.index(upto)

    def din(name, shape, dt=F32):
        return nc.dram_tensor(name, list(shape), dt, kind="ExternalInput").ap()

    def dscr(name, shape, dt):
        return nc.dram_tensor(name, list(shape), dt, kind=("ExternalOutput" if (debug and name in debug) else "Internal")).ap()

    xh = din("xh", [TLOC, D])
    memx = din("memx", [256, D])
    g1bc = din("g1bc", [128, D])
    gmbc = din("gmbc", [128, D])
    g2bc = din("g2bc", [128, D])
    brbc = din("brbc", [128, 72])
    gcols = din("gcols", [128, 4])
    goutc = din("goutc", [128, 16])
    convc = din("convc", [128, 12])
    w_in = din("w_in", [D, 5120])
    w_mem = din("w_mem", [D, 1024])
    w_out = din("w_out", [D, D])
    w_r = din("w_r", [D, 72])
    if lvl >= 4:
        wg = [din("wg%d" % c, [16384, 2048]) for c in range(4)]
        wu = [din("wu%d" % c, [16384, 2048]) for c in range(4)]
        wd = [din("wd%d" % c, [16384, 2048]) for c in range(4)]
    identd = din("ident", [128, 128], BF16)
    triud = din("triu", [128, 128], BF16)
    biasd = din("biasS", [128, 48 * 128], BF16)
    kinvd = din("kinv", [1, TLOC], BF16)
    base8d = din("base8", [128, 8])
    out = nc.dram_tensor("out", [TOWN, D], F32, kind="ExternalOutput").ap()

    KT = dscr("KT", [8, 128, TLOC], BF16)
    VT = dscr("VT", [8, 128, TLOC], BF16)
    QT = dscr("QT", [8, 128, TOWN], BF16)
    YCM = dscr("YCM", [8, 128, TOWN], BF16)
    ATT = dscr("ATT", [8, 128, TOWN], BF16)
    X1 = dscr("X1", [TOWN, D], F32)
    H2 = dscr("H2", [TOWN, D], BF16)
    XBUF = dscr("XBUF", [RBUF, D], BF16)
    YBUF = dscr("YBUF", [RBUF, D], F32)

    try:
      with ExitStack() as top:
        P = Prog(nc, top)
        P.reset_sems()
        dbg_out = {}

        def stop_if(k):
            if lvl != k:
                return
            if not debug:
                P.reset_sems()
            if debug:
                for nm, t_ in dbg_out.items():
                    if nm not in debug:
                        continue
                    shp = list(t_.t.shape)
                    d_ = nc.dram_tensor("dbg_" + nm, shp, t_.t.dtype, kind="ExternalOutput").ap()
                    P.dma("sp", lambda e, d_=d_, t_=t_: e.dma_start(out=d_, in_=t_.t[:]), reads=[t_], writes=[TB()])
                P.end_phase()
            raise _Stop()

        def mk(st, name, shape, dt, psum=False):
            f = nc.psum_tensor if psum else nc.sbuf_tensor
            return T(st.enter_context(f(name, list(shape), dt)))

        ident = mk(top, "ident_s", [128, 128], BF16)
        ones = mk(top, "ones_s", [128, 128], BF16)
        gcol = mk(top, "gcol_s", [128, 4], F32)
        gout = mk(top, "gout_s", [128, 16], F32)
        ssA = mk(top, "ssA_s", [128, NT], F32)
        gates = mk(top, "gates_s", [128, NT, 2], F32)
        desti = mk(top, "desti_s", [128, NT, 2], I32)
        idxW = mk(top, "idxW", [128, NBLK * 8], I32)
        dbg_out["idxW"] = idxW
        P.dma("sp", lambda e: e.dma_start(out=ident.t[:], in_=identd), writes=[ident])
        P.dma("sp", lambda e: e.dma_start(out=gcol.t[:], in_=gcols), writes=[gcol])
        P.dma("sp", lambda e: e.dma_start(out=gout.t[:], in_=goutc), writes=[gout])
        P.op("pool", lambda e: e.memset(ones.t[:], 1.0), writes=[ones])
        dbg_out.update(ssA=ssA, gates=gates, desti=desti)

        def rstd_of(ss_ap, ssT, tmp, tmp_ap, out, out_ap, scale):
            P.op("act", lambda e: e.activation(out=tmp_ap, in_=ss_ap, func=AF.Sqrt, bias=EPS, scale=scale),
                 reads=[ssT], writes=[tmp])
            P.op("dve", lambda e: e.reciprocal(out=out_ap, in_=tmp_ap), reads=[tmp], writes=[out])

        def cast_load_w(Wt, src, ncols, col0, dcol0):
            step = 1024
            for k in range(16):
                for c in range(0, ncols, step):
                    n = min(step, ncols - c)
                    P.dma("pool", lambda e, k=k, c=c, n=n: e.dma_start(
                        out=Wt.t[:, k, dcol0 + c:dcol0 + c + n],
                        in_=src[k * 128:(k + 1) * 128, col0 + c:col0 + c + n]), writes=[Wt])

        with ExitStack() as st:
            gbc = mk(st, "gbc", [128, D], F32)
            xt = [mk(st, "xt%d" % i, [128, D], F32) for i in range(2)]
            junk = mk(st, "junk", [128, D], BF16)
            ss = mk(st, "ss", [128, 1], F32)
            tmp1 = mk(st, "tmp1", [128, 1], F32)
            rstd = mk(st, "rstd", [128, 1], F32)
            xn = mk(st, "xn", [128, D], BF16)
            xnT = mk(st, "xnT", [128, 16, 512], BF16)
            W = mk(st, "W", [128, 16, 3072], BF16)
            mkT = mk(st, "mkT", [128, 4, 256], BF16)
            mv = mk(st, "mv", [128, 2, 512], BF16)
            sq = mk(st, "sq", [128, 512], BF16)
            tf = mk(st, "tf", [128, 512], F32)
            rf = mk(st, "rf", [128, 512], F32)
            ob = [mk(st, "ob%d" % i, [128, 512], BF16) for i in range(2)]
            csb = mk(st, "csb", [128, 512], F32)
            zb = [mk(st, "zb%d" % i, [128, 514], F32) for i in range(4)]
            t1 = mk(st, "t1", [128, 512], F32)
            yc = [mk(st, "yc%d" % i, [128, 512], F32) for i in range(4)]
            pTm = mk(st, "pTm", [128, 2, 512], BF16)
            cw = mk(st, "cw", [128, 12], F32)
            pT = mk(st, "pT", [128, 2048], BF16, psum=True)
            pp = [mk(st, "pp%d" % i, [128, 512], F32, psum=True) for i in range(3)]
            p2 = mk(st, "p2", [128, 512], F32, psum=True)
            pS = [mk(st, "pS%d" % i, [128, 512], F32, psum=True) for i in range(2)]
            P.dma("sp", lambda e: e.dma_start(out=cw.t[:], in_=convc), writes=[cw])
            for i in range(4):
                P.op("pool", lambda e, i=i: e.memset(zb[i].t[:], 0.0), writes=[zb[i]])
            cnt = {"x": 0, "pp": 0, "ob": 0}

            def front(rows_ap_fn, ntiles):
                for ti in range(ntiles):
                    x_ = xt[cnt["x"] % 2]
                    cnt["x"] += 1
                    P.dma("sp", lambda e, x_=x_, ti=ti: e.dma_start(out=x_.t[:], in_=rows_ap_fn(ti)), writes=[x_])
                    P.op("act", lambda e, x_=x_: e.activation(out=junk.t[:], in_=x_.t[:], func=AF.Square,
                                                              accum_out=ss.t[:]), reads=[x_], writes=[junk, ss])
                    rstd_of(ss.t[:], ss, tmp1, tmp1.t[:], rstd, rstd.t[:], 1.0 / D)
                    P.op("dve", lambda e, x_=x_: e.scalar_tensor_tensor(
                        out=xn.t[:], in0=x_.t[:], scalar=rstd.t[:, 0:1], in1=gbc.t[:], op0=ALU.mult, op1=ALU.mult),
                        reads=[x_, rstd, gbc], writes=[xn])

                    def tr(e):
                        for k in range(16):
                            i_ = e.transpose(out=pT.t[:, k * 128:(k + 1) * 128], in_=xn.t[:, k * 128:(k + 1) * 128],
                                             identity=ident.t[:])
                        return i_
                    P.op("pe", tr, reads=[xn, ident], writes=[pT])
                    P.op("act", lambda e, ti=ti: e.activation(
                        out=xnT.t[:, 0:8, ti * 128:(ti + 1) * 128],
                        in_=pT.t[:, 0:1024].rearrange("p (k n) -> p k n", n=128), func=AF.Copy),
                        reads=[pT], writes=[xnT])
                    P.op("dve", lambda e, ti=ti: e.tensor_copy(
                        out=xnT.t[:, 8:16, ti * 128:(ti + 1) * 128],
                        in_=pT.t[:, 1024:2048].rearrange("p (k n) -> p k n", n=128)),
                        reads=[pT], writes=[xnT])

            def proj(col0, ntok=512):
                p_ = pp[cnt["pp"] % 3]
                cnt["pp"] += 1

                def f(e):
                    for k in range(16):
                        i_ = e.matmul(p_.t[:, 0:ntok], lhsT=W.t[:, k, col0:col0 + 128], rhs=xnT.t[:, k, 0:ntok],
                                      start=(k == 0), stop=(k == 15))
                    return i_
                P.op("pe", f, reads=[W, xnT], writes=[p_])
                return p_

            def rep_rstd(srcs, scale, ntok=512):
                n = len(srcs)
                for i, (s_, ap) in enumerate(srcs):
                    P.op("act", lambda e, ap=ap: e.activation(out=sq.t[:, 0:ntok], in_=ap, func=AF.Square),
                         reads=[s_], writes=[sq])
                    P.op("pe", lambda e, i=i: e.matmul(p2.t[:, 0:ntok], lhsT=ones.t[:], rhs=sq.t[:, 0:ntok],
                                                       start=(i == 0), stop=(i == n - 1)),
                         reads=[ones, sq], writes=[p2])
                rstd_of(p2.t[:, 0:ntok], p2, tf, tf.t[:, 0:ntok], rf, rf.t[:, 0:ntok], scale)

            def scaled_out(src, src_ap, gain_ap, gainT, ntok=512):
                o_ = ob[cnt["ob"] % 2]
                cnt["ob"] += 1
                P.op("dve", lambda e: e.scalar_tensor_tensor(
                    out=o_.t[:, 0:ntok], in0=src_ap, scalar=gain_ap, in1=rf.t[:, 0:ntok], op0=ALU.mult,
                    op1=ALU.mult), reads=[src, gainT, rf], writes=[o_])
                return o_

            def store(o_, dst_ap, ntok=512):
                P.dma("sp", lambda e: e.dma_start(out=dst_ap, in_=o_.t[:, 0:ntok]), reads=[o_], writes=[TB()])

            P.dma("sp", lambda e: e.dma_start(out=gbc.t[:], in_=gmbc), writes=[gbc])
            cast_load_w(W, w_mem, 1024, 0, 0)
            front(lambda ti: memx[ti * 128:(ti + 1) * 128, :], 2)
            for hm in range(4):
                p_ = proj(hm * 128, 256)
                rep_rstd([(p_, p_.t[:, 0:256])], 1.0 / 128, 256)
                P.op("dve", lambda e, p_=p_, hm=hm: e.scalar_tensor_tensor(
                    out=mkT.t[:, hm, :], in0=p_.t[:, 0:256], scalar=gcol.t[:, 3:4], in1=rf.t[:, 0:256],
                    op0=ALU.mult, op1=ALU.mult), reads=[p_, gcol, rf], writes=[mkT])
            for mc in range(2):
                p_ = pp[cnt["pp"] % 3]
                cnt["pp"] += 1

                def f(e, p_=p_, mc=mc):
                    for k in range(16):
                        i_ = e.matmul(p_.t[:, :], lhsT=xnT.t[:, k, mc * 128:(mc + 1) * 128], rhs=W.t[:, k, 512:1024],
                                      start=(k == 0), stop=(k == 15))
                    return i_
                P.op("pe", f, reads=[W, xnT], writes=[p_])
                P.op("act", lambda e, p_=p_, mc=mc: e.activation(out=mv.t[:, mc, :], in_=p_.t[:, :], func=AF.Copy),
                     reads=[p_], writes=[mv])

            P.dma("sp", lambda e: e.dma_start(out=gbc.t[:], in_=g1bc), writes=[gbc])
            cast_load_w(W, w_in, 2048, 1024, 0)
            for s_ in range(TLOC // 512):
                front(lambda ti, s_=s_: xh[s_ * 512 + ti * 128:s_ * 512 + (ti + 1) * 128, :], 4)
                for h in range(8):
                    p_ = proj(h * 128)
                    rep_rstd([(p_, p_.t[:, :])], 1.0 / 128)
                    o_ = scaled_out(p_, p_.t[:, :], gcol.t[:, 1:2], gcol)
                    store(o_, KT[h, :, s_ * 512:(s_ + 1) * 512])
                for h in range(8):
                    p_ = proj(1024 + h * 128)
                    o_ = ob[cnt["ob"] % 2]
                    cnt["ob"] += 1
                    P.op("act", lambda e, p_=p_, o_=o_: e.activation(out=o_.t[:, :], in_=p_.t[:, :], func=AF.Copy),
                         reads=[p_], writes=[o_])
                    store(o_, VT[h, :, s_ * 512:(s_ + 1) * 512])

            cast_load_w(W, w_in, 1024, 0, 0)
            cast_load_w(W, w_in, 2048, 3072, 1024)
            for s_ in range(3, TLOC // 512):
                own = s_ >= 4
                oc = (s_ - 4) * 512
                front(lambda ti, s_=s_: xh[s_ * 512 + ti * 128:s_ * 512 + (ti + 1) * 128, :], 4)
                if own:
                    for h in range(8):
                        p_ = proj(h * 128)
                        rep_rstd([(p_, p_.t[:, :])], 1.0 / 128)
                        o_ = scaled_out(p_, p_.t[:, :], gcol.t[:, 0:1], gcol)
                        store(o_, QT[h, :, oc:oc + 512])
                for cc in range(4):
                    pc = proj(1536 + cc * 128)
                    pu = proj(2048 + cc * 128)
                    P.op("act", lambda e, pc=pc: e.activation(out=csb.t[:, :], in_=pc.t[:, :], func=AF.Copy),
                         reads=[pc], writes=[csb])
                    z = zb[cc]
                    P.op("dve", lambda e, z=z, pu=pu: e.tensor_tensor(out=z.t[:, 2:514], in0=csb.t[:, :],
                                                                      in1=pu.t[:, :], op=ALU.mult),
                         reads=[csb, pu], writes=[z])
                    if own:
                        pb = proj(1024 + cc * 128)
                        P.op("dve", lambda e, z=z, cc=cc: e.tensor_scalar(
                            out=t1.t[:, :], in0=z.t[:, 0:512], scalar1=cw.t[:, cc * 3:cc * 3 + 1], scalar2=None,
                            op0=ALU.mult), reads=[z, cw], writes=[t1])
                        P.op("dve", lambda e, z=z, cc=cc: e.scalar_tensor_tensor(
                            out=t1.t[:, :], in0=z.t[:, 1:513], scalar=cw.t[:, cc * 3 + 1:cc * 3 + 2], in1=t1.t[:, :],
                            op0=ALU.mult, op1=ALU.add), reads=[z, cw, t1], writes=[t1])
                        P.op("dve", lambda e, z=z, cc=cc: e.scalar_tensor_tensor(
                            out=t1.t[:, :], in0=z.t[:, 2:514], scalar=cw.t[:, cc * 3 + 2:cc * 3 + 3], in1=t1.t[:, :],
                            op0=ALU.mult, op1=ALU.add), reads=[z, cw, t1], writes=[t1])
                        P.op("dve", lambda e, pb=pb, cc=cc: e.tensor_tensor(out=yc[cc].t[:, :], in0=t1.t[:, :],
                                                                            in1=pb.t[:, :], op=ALU.mult),
                             reads=[t1, pb], writes=[yc[cc]])
                    P.op("dve", lambda e, z=z: e.tensor_copy(out=z.t[:, 0:2], in_=z.t[:, 512:514]),
                         reads=[z], writes=[z])
                if not own:
                    continue
                rep_rstd([(yc[cc], yc[cc].t[:, :]) for cc in range(4)], 1.0 / 512)
                for cc in range(4):
                    o_ = scaled_out(yc[cc], yc[cc].t[:, :], gout.t[:, 8 + cc:9 + cc], gout)
                    store(o_, YCM[cc, :, oc:oc + 512])
                for hm in range(4):
                    p_ = proj(2560 + hm * 128)
                    rep_rstd([(p_, p_.t[:, :])], 1.0 / 128)
                    mq = scaled_out(p_, p_.t[:, :], gcol.t[:, 2:3], gcol)
                    for mc in range(2):
                        P.op("pe", lambda e, mc=mc, hm=hm, mq=mq: e.matmul(
                            pS[mc].t[:, :], lhsT=mkT.t[:, hm, mc * 128:(mc + 1) * 128], rhs=mq.t[:, :],
                            start=True, stop=True), reads=[mkT, mq], writes=[pS[mc]])
                        P.op("act", lambda e, mc=mc: e.activation(out=pTm.t[:, mc, :], in_=pS[mc].t[:, :],
                                                                  func=AF.Exp, scale=SC),
                             reads=[pS[mc]], writes=[pTm])
                    po = pp[cnt["pp"] % 3]
                    cnt["pp"] += 1

                    def fo(e, po=po, hm=hm):
                        for mc in range(2):
                            i_ = e.matmul(po.t[:, :], lhsT=mv.t[:, mc, hm * 128:(hm + 1) * 128], rhs=pTm.t[:, mc, :],
                                          start=(mc == 0), stop=(mc == 1))
                        return i_
                    P.op("pe", fo, reads=[mv, pTm], writes=[po])

                    def fl(e):
                        for mc in range(2):
                            i_ = e.matmul(p2.t[:, :], lhsT=ones.t[:], rhs=pTm.t[:, mc, :], start=(mc == 0),
                                          stop=(mc == 1))
                        return i_
                    P.op("pe", fl, reads=[ones, pTm], writes=[p2])
                    P.op("dve", lambda e: e.reciprocal(out=rf.t[:, :], in_=p2.t[:, :]), reads=[p2], writes=[rf])
                    P.op("dve", lambda e, po=po, hm=hm: e.tensor_tensor(out=yc[hm].t[:, :], in0=po.t[:, :],
                                                                        in1=rf.t[:, :], op=ALU.mult),
                         reads=[po, rf], writes=[yc[hm]])
                rep_rstd([(yc[hm], yc[hm].t[:, :]) for hm in range(4)], 1.0 / 512)
                for hm in range(4):
                    o_ = scaled_out(yc[hm], yc[hm].t[:, :], gout.t[:, 12 + hm:13 + hm], gout)
                    store(o_, YCM[4 + hm, :, oc:oc + 512])
            P.end_phase()
            stop_if(0)

        with ExitStack() as st:
            QTh = mk(st, "QTh", [128, TOWN], BF16)
            KTh = mk(st, "KTh", [128, TLOC], BF16)
            VTh = mk(st, "VTh", [128, TLOC], BF16)
            Vtok = [mk(st, "Vtok%d" % i, [128, 48 * 128], BF16) for i in range(3)]
            accO = mk(st, "accO", [128, TOWN], F32)
            accL = mk(st, "accL", [128, TOWN], F32)
            biasS = mk(st, "biasS_s", [128, 48 * 128], BF16)
            kinv = mk(st, "kinv_s", [1, TLOC], BF16)
            negr = mk(st, "negr", [1, 128], BF16)
            pTs = [mk(st, "pTs%d" % i, [128, 256], BF16) for i in range(2)]
            sqa = mk(st, "sqa", [128, TOWN], BF16)
            psS = [mk(st, "psS%d" % i, [128, 512], F32, psum=True) for i in range(2)]
            psO = [mk(st, "psO%d" % i, [128, 512], F32, psum=True) for i in range(2)]
            psL = [mk(st, "psL%d" % i, [128, 512], F32, psum=True) for i in range(2)]
            pV = mk(st, "pV", [128, 1024], BF16, psum=True)
            psSS = mk(st, "psSS", [128, 512], F32, psum=True)
            P.dma("sp", lambda e: e.dma_start(out=biasS.t[:], in_=biasd), writes=[biasS])
            P.dma("sp", lambda e: e.dma_start(out=kinv.t[:], in_=kinvd), writes=[kinv])
            P.op("pool", lambda e: e.memset(negr.t[:], -1.0e5), writes=[negr])
            nu = 0
            ng = 0
            for h in range(8):
                P.dma("sp", lambda e, h=h: e.dma_start(out=QTh.t[:], in_=QT[h]), writes=[QTh])
                P.dma("sp", lambda e, h=h: e.dma_start(out=KTh.t[:], in_=KT[h]), writes=[KTh])
                P.dma("sp", lambda e, h=h: e.dma_start(out=VTh.t[:], in_=VT[h]), writes=[VTh])
                ev = 0
                for bi, d in enumerate(BRANCH):
                    for i0 in range(0, 48, 8):
                        def ftr(e, i0=i0, d=d):
                            for j in range(8):
                                idx = i0 + j
                                k0 = (idx // d) * 128 * d + (idx % d)
                                i_ = e.transpose(out=pV.t[:, j * 128:(j + 1) * 128], in_=VTh.t[:, ssl(k0, 128, d)],
                                                 identity=ident.t[:])
                            return i_
                        P.op("pe", ftr, reads=[VTh, ident], writes=[pV])
                        if ev % 2 == 0:
                            P.op("act", lambda e, bi=bi, i0=i0: e.activation(
                                out=Vtok[bi].t[:, i0 * 128:(i0 + 8) * 128], in_=pV.t[:, :], func=AF.Copy),
                                reads=[pV], writes=[Vtok[bi]])
                        else:
                            P.op("dve", lambda e, bi=bi, i0=i0: e.tensor_copy(
                                out=Vtok[bi].t[:, i0 * 128:(i0 + 8) * 128], in_=pV.t[:, :]),
                                reads=[pV], writes=[Vtok[bi]])
                        ev += 1
                for bi, d in enumerate(BRANCH):
                    nB0, nB1 = HALO // (128 * d), TLOC // (128 * d)
                    tiles = [(Bq, r) for Bq in range(nB0, nB1) for r in range(d)]
                    for g0 in range(0, len(tiles), 4):
                        grp = tiles[g0:g0 + 4]
                        pO = psO[ng % 2]
                        pL = psL[ng % 2]
                        ng += 1
                        for u, (Bq, r) in enumerate(grp):
                            q0 = Bq * 128 * d + r
                            pS_ = psS[nu % 2]
                            pT_ = pTs[nu % 2]
                            nu += 1

                            def fs(e, q0=q0, d=d, bi=bi, h=h, pS_=pS_):
                                for kt in range(2):
                                    k0 = q0 - (1 - kt) * 128 * d
                                    halo = k0 < HALO
                                    o_ap = pS_.t[:, kt * 128:(kt + 1) * 128]
                                    e.matmul(o_ap, lhsT=KTh.t[:, ssl(k0, 128, d)],
                                             rhs=QTh.t[:, ssl(q0 - HALO, 128, d)], start=True, stop=False)
                                    bc = ((bi * 8 + h) * 2 + kt) * 128
                                    i_ = e.matmul(o_ap, lhsT=ident.t[:], rhs=biasS.t[:, bc:bc + 128], start=False,
                                                  stop=(not halo))
                                    if halo:
                                        i_ = e.matmul(o_ap, lhsT=kinv.t[0:1, ssl(k0, 128, d)], rhs=negr.t[0:1, :],
                                                      start=False, stop=True)
                                return i_
                            P.op("pe", fs, reads=[KTh, QTh, ident, biasS, kinv, negr], writes=[pS_])
                            P.op("act", lambda e, pS_=pS_, pT_=pT_: e.activation(
                                out=pT_.t[:, :], in_=pS_.t[:, 0:256], func=AF.Exp, scale=SC),
                                reads=[pS_], writes=[pT_])

                            def fo(e, q0=q0, d=d, bi=bi, u=u, pT_=pT_, pO=pO, pL=pL):
                                for kt in range(2):
                                    k0 = q0 - (1 - kt) * 128 * d
                                    idx = (k0 // (128 * d)) * d + (k0 % d)
                                    e.matmul(pO.t[:, u * 128:(u + 1) * 128], lhsT=Vtok[bi].t[:, idx * 128:(idx + 1) * 128],
                                             rhs=pT_.t[:, kt * 128:(kt + 1) * 128], start=(kt == 0), stop=(kt == 1))
                                for kt in range(2):
                                    i_ = e.matmul(pL.t[:, u * 128:(u + 1) * 128], lhsT=ones.t[:],
                                                  rhs=pT_.t[:, kt * 128:(kt + 1) * 128], start=(kt == 0),
                                                  stop=(kt == 1))
                                return i_
                            P.op("pe", fo, reads=[Vtok[bi], pT_, ones], writes=[pO, pL])
                        Bq, r0 = grp[0]

                        def view(a):
                            if d == 1:
                                c0 = Bq * 128 - HALO
                                return a.t[:, c0:c0 + 512].rearrange("p (u i) -> p u i", i=128)
                            if d == 4:
                                c0 = Bq * 512 - HALO
                                return a.t[:, c0:c0 + 512].rearrange("p (i u) -> p u i", u=4)
                            c0 = Bq * 2048 - HALO
                            return a.t[:, c0:c0 + 2048].rearrange("p (i r) -> p r i", r=16)[:, r0:r0 + 4, :]
                        vO, vL = view(accO), view(accL)
                        pOv = pO.t[:, :].rearrange("p (u i) -> p u i", i=128)
                        pLv = pL.t[:, :].rearrange("p (u i) -> p u i", i=128)
                        if bi == 0:
                            P.op("act", lambda e, vO=vO, pOv=pOv: e.activation(out=vO, in_=pOv, func=AF.Copy),
                                 reads=[pO], writes=[accO])
                            P.op("dve", lambda e, vL=vL, pLv=pLv: e.tensor_copy(out=vL, in_=pLv),
                                 reads=[pL], writes=[accL])
                        else:
                            P.op("dve", lambda e, vO=vO, pOv=pOv: e.tensor_tensor(out=vO, in0=pOv, in1=vO, op=ALU.add),
                                 reads=[pO, accO], writes=[accO])
                            P.op("dve", lambda e, vL=vL, pLv=pLv: e.tensor_tensor(out=vL, in0=pLv, in1=vL, op=ALU.add),
                                 reads=[pL, accL], writes=[accL])
                P.op("dve", lambda e: e.reciprocal(out=accL.t[:, :], in_=accL.t[:, :]), reads=[accL], writes=[accL])
                P.op("dve", lambda e: e.tensor_tensor(out=accO.t[:, :], in0=accO.t[:, :], in1=accL.t[:, :], op=ALU.mult),
                     reads=[accO, accL], writes=[accO])
                P.op("act", lambda e: e.activation(out=sqa.t[:, :], in_=accO.t[:, :], func=AF.Square),
                     reads=[accO], writes=[sqa])

                def fss(e):
                    for t_ in range(NT):
                        i_ = e.matmul(psSS.t[:, t_:t_ + 1], lhsT=sqa.t[:, t_ * 128:(t_ + 1) * 128], rhs=ones.t[:, 0:1],
                                      start=True, stop=True)
                    return i_
                P.op("pe", fss, reads=[sqa, ones], writes=[psSS])
                if h == 0:
                    P.op("dve", lambda e: e.tensor_copy(out=ssA.t[:, :], in_=psSS.t[:, 0:NT]), reads=[psSS], writes=[ssA])
                else:
                    P.op("dve", lambda e: e.tensor_tensor(out=ssA.t[:, :], in0=psSS.t[:, 0:NT], in1=ssA.t[:, :],
                                                          op=ALU.add), reads=[psSS, ssA], writes=[ssA])
                P.op("act", lambda e, h=h: e.activation(out=sqa.t[:, :], in_=accO.t[:, :], func=AF.Copy,
                                                        scale=gout.t[:, h:h + 1]), reads=[accO, gout], writes=[sqa])
                P.dma("sp", lambda e, h=h: e.dma_start(out=ATT[h], in_=sqa.t[:, :]), reads=[sqa], writes=[TB()])
            P.end_phase()
            stop_if(1)

        with ExitStack() as st2:
            A0 = mk(st2, "A0", [128, NT, 64], BF16)
            A1 = mk(st2, "A1", [128, NT, 64], BF16)
            Ab = mk(st2, "Ab", [128, NT, 64], BF16)
            with ExitStack() as st:
                Wo = mk(st, "Wo", [128, 16, D], BF16)
                Wr = mk(st, "Wr", [128, 16, 72], BF16)
                gbc = mk(st, "gbc2", [128, D], F32)
                brb = mk(st, "brb", [128, 72], F32)
                rstdA = mk(st, "rstdA", [128, NT], F32)
                tmpA = mk(st, "tmpA", [128, NT], F32)
                aT = [mk(st, "aT%d" % i, [128, 8, 128], BF16) for i in range(2)]
                yT = [mk(st, "yT%d" % i, [128, 8, 128], BF16) for i in range(2)]
                xt = [mk(st, "xt2_%d" % i, [128, D], F32) for i in range(2)]
                x1 = [mk(st, "x1_%d" % i, [128, D], F32) for i in range(2)]
                junk = mk(st, "junk2", [128, D], BF16)
                ss = mk(st, "ss2", [128, 1], F32)
                tmp1 = mk(st, "tmp2", [128, 1], F32)
                rstd = mk(st, "rstd2", [128, 1], F32)
                h2 = [mk(st, "h2_%d" % i, [128, D], BF16) for i in range(2)]
                h2T = mk(st, "h2T", [128, 16, 128], BF16)
                lg = mk(st, "lg", [128, 72], F32)
                m8 = mk(st, "m8", [128, 8], F32)
                m8e = mk(st, "m8e", [128, 8], F32)
                ohg = mk(st, "ohg", [128, 8], F32)
                oh = [mk(st, "oh%d" % i, [128, 8], F32) for i in range(2)]
                le = mk(st, "le", [128, 8], F32)
                sm = mk(st, "sm", [128, 8], F32)
                pA = [mk(st, "pA%d" % i, [128, 512], F32, psum=True) for i in range(2)]
                pB = [mk(st, "pB%d" % i, [128, 512], F32, psum=True) for i in range(2)]
                pT = mk(st, "pT3", [128, 2048], BF16, psum=True)
                pR = mk(st, "pR", [128, 512], F32, psum=True)
                cast_load_w(Wo, w_out, D, 0, 0)
                for k in range(16):
                    P.dma("pool", lambda e, k=k: e.dma_start(out=Wr.t[:, k, :], in_=w_r[k * 128:(k + 1) * 128, :]),
                          writes=[Wr])
                P.dma("sp", lambda e: e.dma_start(out=gbc.t[:], in_=g2bc), writes=[gbc])
                P.dma("sp", lambda e: e.dma_start(out=brb.t[:], in_=brbc), writes=[brb])
                rstd_of(ssA.t[:, :], ssA, tmpA, tmpA.t[:, :], rstdA, rstdA.t[:, :], 1.0 / 1024)
                ATTv = ATT.rearrange("c p n -> p c n")
                YCMv = YCM.rearrange("c p n -> p c n")
                npp = 0
                for t_ in range(NT):
                    a_, y_, x_, x1_, h2_ = aT[t_ % 2], yT[t_ % 2], xt[t_ % 2], x1[t_ % 2], h2[t_ % 2]
                    c0 = t_ * 128
                    P.dma("sp", lambda e, a_=a_, c0=c0: e.dma_start(out=a_.t[:], in_=ATTv[:, :, c0:c0 + 128]), writes=[a_])
                    P.dma("sp", lambda e, y_=y_, c0=c0: e.dma_start(out=y_.t[:], in_=YCMv[:, :, c0:c0 + 128]), writes=[y_])
                    P.dma("sp", lambda e, x_=x_, c0=c0: e.dma_start(out=x_.t[:], in_=xh[HALO + c0:HALO + c0 + 128, :]),
                          writes=[x_])
                    for nb in range(4):
                        pa, pb_ = pA[npp % 2], pB[npp % 2]
                        npp += 1

                        def fa(e, pa=pa, a_=a_, nb=nb):
                            for c in range(8):
                                i_ = e.matmul(pa.t[:, :], lhsT=a_.t[:, c, :], rhs=Wo.t[:, c, nb * 512:(nb + 1) * 512],
                                              start=(c == 0), stop=(c == 7))
                            return i_
                        P.op("pe", fa, reads=[a_, Wo], writes=[pa])

                        def fb(e, pb_=pb_, y_=y_, nb=nb):
                            for c in range(8):
                                i_ = e.matmul(pb_.t[:, :], lhsT=y_.t[:, c, :], rhs=Wo.t[:, 8 + c, nb * 512:(nb + 1) * 512],
                                              start=(c == 0), stop=(c == 7))
                            return i_
                        P.op("pe", fb, reads=[y_, Wo], writes=[pb_])
                        P.op("dve", lambda e, pa=pa, x_=x_, x1_=x1_, nb=nb, t_=t_: e.scalar_tensor_tensor(
                            out=x1_.t[:, nb * 512:(nb + 1) * 512], in0=pa.t[:, :], scalar=rstdA.t[:, t_:t_ + 1],
                            in1=x_.t[:, nb * 512:(nb + 1) * 512], op0=ALU.mult, op1=ALU.add),
                            reads=[pa, rstdA, x_], writes=[x1_])
                        P.op("dve", lambda e, pb_=pb_, x1_=x1_, nb=nb: e.tensor_tensor(
                            out=x1_.t[:, nb * 512:(nb + 1) * 512], in0=pb_.t[:, :], in1=x1_.t[:, nb * 512:(nb + 1) * 512],
                            op=ALU.add), reads=[pb_, x1_], writes=[x1_])
                    P.dma("sp", lambda e, x1_=x1_, c0=c0: e.dma_start(out=X1[c0:c0 + 128, :], in_=x1_.t[:]),
                          reads=[x1_], writes=[TB()])
                    P.op("act", lambda e, x1_=x1_: e.activation(out=junk.t[:], in_=x1_.t[:], func=AF.Square,
                                                                accum_out=ss.t[:]), reads=[x1_], writes=[junk, ss])
                    rstd_of(ss.t[:], ss, tmp1, tmp1.t[:], rstd, rstd.t[:], 1.0 / D)
                    P.op("dve", lambda e, x1_=x1_, h2_=h2_: e.scalar_tensor_tensor(
                        out=h2_.t[:], in0=x1_.t[:], scalar=rstd.t[:, 0:1], in1=gbc.t[:], op0=ALU.mult, op1=ALU.mult),
                        reads=[x1_, rstd, gbc], writes=[h2_])
                    P.dma("sp", lambda e, h2_=h2_, c0=c0: e.dma_start(out=H2[c0:c0 + 128, :], in_=h2_.t[:]),
                          reads=[h2_], writes=[TB()])

                    def tr(e, h2_=h2_):
                        for k in range(16):
                            i_ = e.transpose(out=pT.t[:, k * 128:(k + 1) * 128], in_=h2_.t[:, k * 128:(k + 1) * 128],
                                             identity=ident.t[:])
                        return i_
                    P.op("pe", tr, reads=[h2_, ident], writes=[pT])
                    P.op("act", lambda e: e.activation(out=h2T.t[:, 0:8, :],
                                                       in_=pT.t[:, 0:1024].rearrange("p (k n) -> p k n", n=128),
                                                       func=AF.Copy), reads=[pT], writes=[h2T])
                    P.op("dve", lambda e: e.tensor_copy(out=h2T.t[:, 8:16, :],
                                                        in_=pT.t[:, 1024:2048].rearrange("p (k n) -> p k n", n=128)),
                         reads=[pT], writes=[h2T])

                    def fr(e):
                        for k in range(16):
                            i_ = e.matmul(pR.t[:, 0:72], lhsT=h2T.t[:, k, :], rhs=Wr.t[:, k, :], start=(k == 0),
                                          stop=(k == 15))
                        return i_
                    P.op("pe", fr, reads=[h2T, Wr], writes=[pR])
                    P.op("dve", lambda e: e.tensor_tensor(out=lg.t[:, :], in0=pR.t[:, 0:72], in1=brb.t[:, :], op=ALU.add),
                         reads=[pR, brb], writes=[lg])
                    P.op("dve", lambda e: e.max(out=m8.t[:, :], in_=lg.t[:, 0:8]), reads=[lg], writes=[m8])
                    P.op("dve", lambda e: e.tensor_scalar(out=ohg.t[:, :], in0=lg.t[:, 0:8], scalar1=m8.t[:, 0:1],
                                                          scalar2=None, op0=ALU.is_equal), reads=[lg, m8], writes=[ohg])
                    P.op("dve", lambda e: e.tensor_scalar(out=sm.t[:, 0:1], in0=m8.t[:, 0:1], scalar1=-1.0, scalar2=None,
                                                          op0=ALU.mult), reads=[m8], writes=[sm])
                    P.op("act", lambda e: e.activation(out=le.t[:, :], in_=lg.t[:, 0:8], func=AF.Exp, bias=sm.t[:, 0:1],
                                                       scale=1.0, accum_out=sm.t[:, 1:2]), reads=[lg, sm], writes=[le, sm])
                    P.op("dve", lambda e: e.reciprocal(out=sm.t[:, 2:3], in_=sm.t[:, 1:2]), reads=[sm], writes=[sm])
                    for g in range(8):
                        if g == 0:
                            P.op("dve", lambda e: e.tensor_scalar(out=le.t[:, :], in0=lg.t[:, 8:16], scalar1=ohg.t[:, 0:1],
                                                                  scalar2=None, op0=ALU.mult), reads=[lg, ohg], writes=[le])
                        else:
                            P.op("dve", lambda e, g=g: e.scalar_tensor_tensor(
                                out=le.t[:, :], in0=lg.t[:, 8 + g * 8:16 + g * 8], scalar=ohg.t[:, g:g + 1], in1=le.t[:, :],
                                op0=ALU.mult, op1=ALU.add), reads=[lg, ohg, le], writes=[le])
                    P.op("dve", lambda e: e.max(out=m8e.t[:, :], in_=le.t[:, :]), reads=[le], writes=[m8e])
                    for k_ in range(2):
                        P.op("dve", lambda e, k_=k_: e.tensor_scalar(out=oh[k_].t[:, :], in0=le.t[:, :],
                                                                     scalar1=m8e.t[:, k_:k_ + 1], scalar2=None,
                                                                     op0=ALU.is_equal), reads=[le, m8e], writes=[oh[k_]])
                    P.op("dve", lambda e: e.tensor_tensor(out=sm.t[:, 3:4], in0=m8e.t[:, 1:2], in1=m8e.t[:, 0:1],
                                                          op=ALU.subtract), reads=[m8e], writes=[sm])
                    P.op("act", lambda e: e.activation(out=sm.t[:, 4:5], in_=sm.t[:, 3:4], func=AF.Exp),
                         reads=[sm], writes=[sm])
                    P.op("dve", lambda e: e.tensor_scalar(out=sm.t[:, 5:6], in0=sm.t[:, 4:5], scalar1=1.0, scalar2=None,
                                                          op0=ALU.add), reads=[sm], writes=[sm])
                    P.op("dve", lambda e: e.reciprocal(out=sm.t[:, 6:7], in_=sm.t[:, 5:6]), reads=[sm], writes=[sm])
                    P.op("dve", lambda e, t_=t_: e.tensor_tensor(out=gates.t[:, t_, 0:1], in0=sm.t[:, 6:7], in1=sm.t[:, 2:3],
                                                                 op=ALU.mult), reads=[sm], writes=[gates])
                    P.op("dve", lambda e, t_=t_: e.tensor_tensor(out=gates.t[:, t_, 1:2], in0=sm.t[:, 2:3],
                                                                 in1=gates.t[:, t_, 0:1], op=ALU.subtract),
                         reads=[sm, gates], writes=[gates])
                    for k_, A_ in enumerate((A0, A1)):
                        for g in range(8):
                            P.op("dve", lambda e, k_=k_, A_=A_, g=g, t_=t_: e.tensor_scalar(
                                out=A_.t[:, t_, g * 8:(g + 1) * 8], in0=oh[k_].t[:, :], scalar1=ohg.t[:, g:g + 1],
                                scalar2=None, op0=ALU.mult), reads=[oh[k_], ohg], writes=[A_])
                    P.op("dve", lambda e, t_=t_: e.tensor_tensor(out=Ab.t[:, t_, :], in0=A0.t[:, t_, :], in1=A1.t[:, t_, :],
                                                                 op=ALU.add), reads=[A0, A1], writes=[Ab])
                P.end_phase()
                stop_if(2)

            with ExitStack() as st:
                triu = mk(st, "triu_s", [128, 128], BF16)
                base8 = mk(st, "base8_s", [128, 8], F32)
                cntf = mk(st, "cntf", [128, 64], F32)
                ci = mk(st, "ci", [128, 64], I32)
                padf = mk(st, "padf", [128, 64], F32)
                zer = mk(st, "zer", [128, 64], F32)
                pends = mk(st, "pends", [128, 64], F32)
                pstart = mk(st, "pstart", [128, 64], F32)
                jk = mk(st, "jk", [128, 64], F32)
                be = mk(st, "be", [128, NBLK], F32)
                same = mk(st, "same", [128, NBLK], F32)
                idxf = mk(st, "idxf", [128, NBLK, 8], F32)
                Acum = mk(st, "Acum", [128, 64], BF16)
                basef = mk(st, "basef", [128, 64], F32)
                dstf = mk(st, "dstf", [128, 2], F32)
                h2t = [mk(st, "h2t%d" % i, [128, D], BF16) for i in range(2)]
                pC = mk(st, "pC", [128, 512], F32, psum=True)
                pK = [mk(st, "pK%d" % i, [128, 512], F32, psum=True) for i in range(2)]
                P.dma("sp", lambda e: e.dma_start(out=triu.t[:], in_=triud), writes=[triu])
                P.dma("sp", lambda e: e.dma_start(out=base8.t[:], in_=base8d), writes=[base8])
                P.op("pool", lambda e: e.memset(zer.t[:], 0.0), writes=[zer])
                P.op("pool", lambda e: e.memset(Acum.t[:], 0.0), writes=[Acum])

                def fc(e):
                    for t_ in range(NT):
                        i_ = e.matmul(pC.t[:, 0:64], lhsT=ones.t[:], rhs=Ab.t[:, t_, :], start=(t_ == 0),
                                      stop=(t_ == NT - 1))
                    return i_
                P.op("pe", fc, reads=[ones, Ab], writes=[pC])
                P.op("dve", lambda e: e.tensor_scalar(out=ci.t[:, :], in0=pC.t[:, 0:64], scalar1=127.0, scalar2=None,
                                                      op0=ALU.add), reads=[pC], writes=[ci])
                P.op("dve", lambda e: e.tensor_single_scalar(out=ci.t[:, :], in_=ci.t[:, :], scalar=-128,
                                                             op=ALU.bitwise_and), reads=[ci], writes=[ci])
                P.op("dve", lambda e: e.tensor_copy(out=padf.t[:, :], in_=ci.t[:, :]), reads=[ci], writes=[padf])
                P.op("dve", lambda e: e.tensor_tensor_scan(out=pends.t[:, :], data0=padf.t[:, :], data1=zer.t[:, :],
                                                           initial=0.0, op0=ALU.add, op1=ALU.add),
                     reads=[padf, zer], writes=[pends])
                P.op("dve", lambda e: e.tensor_tensor(out=pstart.t[:, :], in0=pends.t[:, :], in1=padf.t[:, :],
                                                      op=ALU.subtract), reads=[pends, padf], writes=[pstart])
                for b in range(NBLK):
                    P.op("dve", lambda e, b=b: e.tensor_scalar(out=jk.t[:, :], in0=pends.t[:, :], scalar1=float(128 * b),
                                                               scalar2=None, op0=ALU.is_le, op1=ALU.add,
                                                               accum_out=be.t[:, b:b + 1]), reads=[pends], writes=[jk, be])
                P.op("pool", lambda e: e.memset(same.t[:, :], 0.0), writes=[same])
                P.op("dve", lambda e: e.tensor_tensor(out=same.t[:, 1:NBLK], in0=be.t[:, 1:NBLK], in1=be.t[:, 0:NBLK - 1],
                                                      op=ALU.is_equal), reads=[be], writes=[same])
                P.op("dve", lambda e: e.scalar_tensor_tensor(out=same.t[:, :], in0=same.t[:, :], scalar=64.0,
                                                             in1=be.t[:, :], op0=ALU.mult, op1=ALU.add),
                     reads=[same, be], writes=[same])
                P.op("dve", lambda e: e.tensor_scalar(out=be.t[:, :], in0=same.t[:, :], scalar1=256.0, scalar2=None,
                                                      op0=ALU.mult), reads=[same], writes=[be])
                for b in range(NBLK):
                    P.op("dve", lambda e, b=b: e.tensor_scalar(out=idxf.t[:, b, :], in0=base8.t[:, :],
                                                               scalar1=be.t[:, b:b + 1], scalar2=None, op0=ALU.add),
                         reads=[base8, be], writes=[idxf])
                P.op("dve", lambda e: e.tensor_copy(out=idxW.t[:, :], in_=idxf.t[:, :, :].rearrange("p b j -> p (b j)")), reads=[idxf], writes=[idxW])
                for t_ in range(NT):
                    pk = pK[t_ % 2]
                    h_ = h2t[t_ % 2]
                    P.dma("sp", lambda e, h_=h_, t_=t_: e.dma_start(out=h_.t[:], in_=H2[t_ * 128:(t_ + 1) * 128, :]),
                          writes=[h_])

                    def fk(e, pk=pk, t_=t_):
                        e.matmul(pk.t[:, 0:64], lhsT=triu.t[:], rhs=Ab.t[:, t_, :], start=True, stop=False)
                        return e.matmul(pk.t[:, 0:64], lhsT=ones.t[:], rhs=Acum.t[:, :], start=False, stop=True)
                    P.op("pe", fk, reads=[triu, ones, Ab, Acum], writes=[pk])
                    P.op("dve", lambda e, t_=t_: e.tensor_tensor(out=Acum.t[:, :], in0=Acum.t[:, :], in1=Ab.t[:, t_, :],
                                                                 op=ALU.add), reads=[Acum, Ab], writes=[Acum])
                    P.op("dve", lambda e, pk=pk: e.tensor_tensor(out=basef.t[:, :], in0=pk.t[:, 0:64], in1=pstart.t[:, :],
                                                                 op=ALU.add), reads=[pk, pstart], writes=[basef])
                    for k_, A_ in enumerate((A0, A1)):
                        P.op("dve", lambda e, A_=A_, t_=t_: e.tensor_tensor(out=jk.t[:, :], in0=basef.t[:, :],
                                                                            in1=A_.t[:, t_, :], op=ALU.mult),
                             reads=[basef, A_], writes=[jk])
                        P.op("dve", lambda e, k_=k_: e.tensor_reduce(out=dstf.t[:, k_:k_ + 1], in_=jk.t[:, :], axis=AX.X,
                                                                     op=ALU.add), reads=[jk], writes=[dstf])
                    P.op("dve", lambda e, t_=t_: e.tensor_copy(out=desti.t[:, t_, :], in_=dstf.t[:, :]),
                         reads=[dstf], writes=[desti])
                    for k_ in range(2):
                        P.dma("pool", lambda e, h_=h_, t_=t_, k_=k_: e.indirect_dma_start(
                            out=XBUF[:, :], out_offset=bass.IndirectOffsetOnAxis(ap=desti.t[:, t_, k_:k_ + 1], axis=0),
                            in_=h_.t[:, :], in_offset=None), reads=[h_, desti], writes=[TB()])
                P.end_phase()
                stop_if(3)

        with ExitStack() as st:
            NSLOT = 3
            _br = {}

            def breg(e):
                if "r" not in _br:
                    _br["r"] = e.to_reg(16383)
                return _br["r"]
            slot = [mk(st, "slot%d" % i, [128, 8 * 2048], BF16) for i in range(NSLOT)]
            xb = [mk(st, "xb%d" % i, [128, D], BF16) for i in range(2)]
            xbT = mk(st, "xbT", [128, 16, 128], BF16)
            sil = mk(st, "sil", [128, 512], F32)
            act = mk(st, "act", [128, 1024], BF16)
            actT = mk(st, "actT", [128, 8, 128], BF16)
            yb = [mk(st, "yb%d" % i, [128, D], F32) for i in range(2)]
            dbg_out.update(xb0=xb[0], xb1=xb[1], yb0=yb[0], yb1=yb[1])
            pT = mk(st, "pT4", [128, 2048], BF16, psum=True)
            pG = [mk(st, "pG%d" % i, [128, 512], F32, psum=True) for i in range(2)]
            pU = [mk(st, "pU%d" % i, [128, 512], F32, psum=True) for i in range(2)]
            pY = [mk(st, "pY%d" % i, [128, 512], F32, psum=True) for i in range(2)]
            ns = 0
            ng = 0
            ny = 0
            for b in range(_DBG.get("nblk", NBLK)):
                xb_ = xb[b % 2]
                P.dma("sp", lambda e, xb_=xb_, b=b: e.dma_start(out=xb_.t[:], in_=XBUF[b * 128:(b + 1) * 128, :]),
                      writes=[xb_])
                ws = []
                for wsrc in (wg, wu, wd):
                    s_ = slot[ns % NSLOT]
                    ns += 1
                    if "w" in _DBG.get("skip", ()):
                        P.op("pool", lambda e, s_=s_: e.memset(s_.t[:], 0.01), writes=[s_])
                    for j in range(0 if "w" in _DBG.get("skip", ()) else 8):
                        P.dma("pool", lambda e, s_=s_, j=j, b=b, wsrc=wsrc: e.indirect_dma_start(
                            out=s_.t[:, j * 2048:(j + 1) * 2048], out_offset=None, in_=wsrc[j % 4][:, :],
                            in_offset=bass.IndirectOffsetOnAxis(ap=idxW.t[:, b * 8 + j:b * 8 + j + 1], axis=0),
                            bounds_check=breg(e), oob_is_err=False),
                            reads=[idxW], writes=[s_])
                    ws.append(s_)
                sg, su, sd = ws

                def tr(e, xb_=xb_):
                    for k in range(16):
                        i_ = e.transpose(out=pT.t[:, k * 128:(k + 1) * 128], in_=xb_.t[:, ssl(k, 128, 16)],
                                         identity=ident.t[:])
                    return i_
                P.op("pe", tr, reads=[xb_, ident], writes=[pT])
                P.op("act", lambda e: e.activation(out=xbT.t[:, 0:8, :],
                                                   in_=pT.t[:, 0:1024].rearrange("p (k n) -> p k n", n=128), func=AF.Copy),
                     reads=[pT], writes=[xbT])
                P.op("dve", lambda e: e.tensor_copy(out=xbT.t[:, 8:16, :],
                                                    in_=pT.t[:, 1024:2048].rearrange("p (k n) -> p k n", n=128)),
                     reads=[pT], writes=[xbT])
                for hf in range(2):
                    pg, pu = pG[ng % 2], pU[ng % 2]
                    ng += 1
                    for (pp_, sw) in ((pg, sg), (pu, su)):
                        def fm(e, pp_=pp_, sw=sw, hf=hf):
                            for k in range(16):
                                c0 = (k // 2) * 2048 + (k % 2) * 1024 + hf * 512
                                i_ = e.matmul(pp_.t[:, :], lhsT=xbT.t[:, k, :], rhs=sw.t[:, c0:c0 + 512],
                                              start=(k == 0), stop=(k == 15))
                            return i_
                        P.op("pe", fm, reads=[xbT, sw], writes=[pp_])
                    P.op("act", lambda e, pg=pg: e.activation(out=sil.t[:, :], in_=pg.t[:, :], func=(AF.Copy if "silu" in _DBG.get("skip", ()) else AF.Silu)),
                         reads=[pg], writes=[sil])
                    P.op("dve", lambda e, pu=pu, hf=hf: e.tensor_tensor(out=act.t[:, hf * 512:(hf + 1) * 512],
                                                                        in0=sil.t[:, :], in1=pu.t[:, :], op=ALU.mult),
                         reads=[sil, pu], writes=[act])

                def tr2(e):
                    for k in range(8):
                        i_ = e.transpose(out=pT.t[:, k * 128:(k + 1) * 128], in_=act.t[:, ssl(k, 128, 8)],
                                         identity=ident.t[:])
                    return i_
                P.op("pe", tr2, reads=[act, ident], writes=[pT])
                P.op("act", lambda e: e.activation(out=actT.t[:, :, :],
                                                   in_=pT.t[:, 0:1024].rearrange("p (k n) -> p k n", n=128), func=AF.Copy),
                     reads=[pT], writes=[actT])
                yb_ = yb[b % 2]
                for nb in range(4):
                    py = pY[ny % 2]
                    ny += 1

                    def fd(e, py=py, sd=sd, nb=nb):
                        for k in range(8):
                            i_ = e.matmul(py.t[:, :], lhsT=actT.t[:, k, :], rhs=sd.t[:, k * 2048 + nb * 512:k * 2048 + (nb + 1) * 512],
                                          start=(k == 0), stop=(k == 7))
                        return i_
                    P.op("pe", fd, reads=[actT, sd], writes=[py])
                    if nb % 2 == 0:
                        P.op("act", lambda e, py=py, yb_=yb_, nb=nb: e.activation(
                            out=yb_.t[:, nb * 512:(nb + 1) * 512], in_=py.t[:, :], func=AF.Copy), reads=[py], writes=[yb_])
                    else:
                        P.op("dve", lambda e, py=py, yb_=yb_, nb=nb: e.tensor_copy(
                            out=yb_.t[:, nb * 512:(nb + 1) * 512], in_=py.t[:, :]), reads=[py], writes=[yb_])
                P.dma("sp", lambda e, yb_=yb_, b=b: e.dma_start(out=YBUF[b * 128:(b + 1) * 128, :], in_=yb_.t[:]),
                      reads=[yb_], writes=[TB()])
            P.end_phase()
            stop_if(4)

        with ExitStack() as st:
            y0 = [mk(st, "y0_%d" % i, [128, D], F32) for i in range(2)]
            y1 = [mk(st, "y1_%d" % i, [128, D], F32) for i in range(2)]
            xo = [mk(st, "xo_%d" % i, [128, D], F32) for i in range(2)]
            for t_ in range(NT):
                a_, b_, x_ = y0[t_ % 2], y1[t_ % 2], xo[t_ % 2]
                P.dma("sp", lambda e, x_=x_, t_=t_: e.dma_start(out=x_.t[:], in_=X1[t_ * 128:(t_ + 1) * 128, :]), writes=[x_])
                for k_, y_ in enumerate((a_, b_)):
                    P.dma("pool", lambda e, y_=y_, t_=t_, k_=k_: e.indirect_dma_start(
                        out=y_.t[:, :], out_offset=None, in_=YBUF[:, :],
                        in_offset=bass.IndirectOffsetOnAxis(ap=desti.t[:, t_, k_:k_ + 1], axis=0)),
                        reads=[desti], writes=[y_])
                P.op("dve", lambda e, a_=a_, x_=x_, t_=t_: e.scalar_tensor_tensor(
                    out=x_.t[:], in0=a_.t[:], scalar=gates.t[:, t_, 0:1], in1=x_.t[:], op0=ALU.mult, op1=ALU.add),
                    reads=[a_, gates, x_], writes=[x_])
                P.op("dve", lambda e, b_=b_, x_=x_, t_=t_: e.scalar_tensor_tensor(
                    out=x_.t[:], in0=b_.t[:], scalar=gates.t[:, t_, 1:2], in1=x_.t[:], op0=ALU.mult, op1=ALU.add),
                    reads=[b_, gates, x_], writes=[x_])
                P.dma("sp", lambda e, x_=x_, t_=t_: e.dma_start(out=out[t_ * 128:(t_ + 1) * 128, :], in_=x_.t[:]),
                      reads=[x_], writes=[TB()])
            P.end_phase()
            stop_if(5)
    except _Stop:
        pass
    return nc


def _consts():
    ident = np.eye(128, dtype=np.float32).astype(BF)
    p = np.arange(128)
    triu = (p[:, None] < p[None, :]).astype(np.float32).astype(BF)
    slopes = 2.0 ** (-8.0 * np.arange(1, 9) / 8.0)
    bias = np.zeros((128, 48, 128), np.float32)
    j = p[:, None]
    i = p[None, :]
    for bi, d in enumerate(BRANCH):
        for h in range(8):
            for kt in range(2):
                dist = i + 128 - j if kt == 0 else i - j
                valid = (dist >= 0) & (dist <= 128)
                b = -slopes[h] * d * dist * (128.0 ** 0.5)
                bias[:, (bi * 8 + h) * 2 + kt, :] = np.where(valid, b, -1.0e5)
    base8 = (p[:, None] * 2 + (np.arange(8) // 4)[None, :]).astype(np.float32)
    return ident, triu, bias.reshape(128, 48 * 128).astype(BF), base8


def _cols(v, n):
    return np.ascontiguousarray(np.asarray(v, np.float32).reshape(n, 128).T)


_NC_CACHE = {}
_DBG = {}


def kernel(x, mem, norm1_g, w_in, q_norm_g, k_norm_g, conv_w, mem_norm_g, w_mem_kv, mem_q_norm_g, mem_k_norm_g,
           out_norm_g, w_out, norm2_g, w_router_group, b_router_group, w_router_expert, b_router_expert,
           w_gate, w_up, w_down):
    f = lambda a: np.asarray(a, np.float32)
    x = f(x)
    mem = f(mem)
    ident, triu, biasS, base8 = _consts()
    rep = lambda v: np.ascontiguousarray(np.broadcast_to(f(v).reshape(1, -1), (128, f(v).size)))
    shared = {
        "g1bc": rep(norm1_g[0]), "gmbc": rep(mem_norm_g[0]), "g2bc": rep(norm2_g[0]),
        "brbc": rep(np.concatenate([f(b_router_group[0]), f(b_router_expert[0])])),
        "gcols": np.ascontiguousarray(np.stack([f(q_norm_g[0]), f(k_norm_g[0]), f(mem_q_norm_g[0]),
                                                f(mem_k_norm_g[0])], axis=1)),
        "goutc": _cols(out_norm_g[0], 16),
        "convc": np.ascontiguousarray(f(conv_w[0]).reshape(3, 4, 128).transpose(2, 1, 0).reshape(128, 12)),
        "w_in": np.ascontiguousarray(f(w_in[0])), "w_mem": np.ascontiguousarray(f(w_mem_kv[0])),
        "w_out": np.ascontiguousarray(f(w_out[0])),
        "w_r": np.ascontiguousarray(np.concatenate([f(w_router_group[0]), f(w_router_expert[0])], axis=1)),
        "ident": ident, "triu": triu, "biasS": biasS, "base8": base8,
    }
    for nm, w_ in (("wg", w_gate), ("wu", w_up), ("wd", w_down)):
        w4 = f(w_[0]).reshape(16384, 4, 2048)
        for q in range(4):
            shared["%s%d" % (nm, q)] = np.ascontiguousarray(w4[:, q, :])
    in_maps = []
    for c in range(NCORE):
        b, s0 = c // 4, (c % 4) * TOWN
        xhc = np.zeros((TLOC, D), np.float32)
        if s0 > 0:
            xhc[:HALO] = x[b, s0 - HALO:s0]
        xhc[HALO:] = x[b, s0:s0 + TOWN]
        kinv = np.zeros((1, TLOC), np.float32)
        if s0 == 0:
            kinv[0, :HALO] = 1.0
        m = dict(shared)
        m["xh"] = xhc
        m["memx"] = np.ascontiguousarray(mem[b])
        m["kinv"] = kinv.astype(BF)
        in_maps.append(m)
    if _DBG.get("upto"):
        lv = ["A1", "A2", "A3", "M2", "M3", "M4"].index(_DBG["upto"])
        if lv < 4:
            for m in in_maps:
                for k_ in [a_ + str(q_) for a_ in ("wg", "wu", "wd") for q_ in range(4)]:
                    m.pop(k_)
        nc = build_nc(_DBG["upto"], debug=_DBG["expose"])
        res = run_bass_kernel_spmd(nc, in_maps, core_ids=list(range(NCORE)))
        _DBG["res"] = res.results
        _DBG["in_maps"] = in_maps
        return None
    if "nc" not in _NC_CACHE:
        _NC_CACHE["nc"] = build_nc()
    res = run_bass_kernel_spmd(_NC_CACHE["nc"], in_maps, core_ids=list(range(NCORE)))
    outp = np.empty((2, 16384, D), np.float32)
    for c in range(NCORE):
        b, s0 = c // 4, (c % 4) * TOWN
        outp[b, s0:s0 + TOWN] = res.results[c]["out"]
    return outp
```
